# Optimizing a Trainium2 kernel written in Bass

```python
import math
import jax, jax.numpy as jnp
from jax import lax
import numpy as np

D_MODEL = 2048
BATCH = 4
SEQ = 4096
DEPTH = 2

CHUNK = 64
Q_BLOCK = 128
HEAD_DIM = 128
SB_HEADS = 6
SB_WIDTH = SB_HEADS * HEAD_DIM
DA_HEADS = 6
DA_HALF = HEAD_DIM // 2
DA_WIDTH = DA_HEADS * HEAD_DIM
CONV_WIDTH = D_MODEL - SB_WIDTH - DA_WIDTH
CONV_GROUPS = CONV_WIDTH // HEAD_DIM
CONV_KERNEL = 31
IN_WIDTH = 3 * SB_WIDTH + 2 * CONV_WIDTH + 3 * DA_WIDTH
D_FF = 5632
N_EXPERTS = 8
TOP_K = 2
D_FF_EXPERT = 2816
EPS = 1e-6
N_DENSE = (DEPTH + 1) // 2
N_MOE = DEPTH // 2

kernel_name = "hybrid_sb_conformer_diffattn_moe"


def rms_norm(x, g):
    xf = x.astype(jnp.float32)
    y = xf * lax.rsqrt(jnp.mean(xf * xf, axis=-1, keepdims=True) + EPS)
    return (y * g.astype(jnp.float32)).astype(x.dtype)


def to_heads(t, n_heads, d):
    b, s, _ = t.shape
    return t.reshape(b, s, n_heads, d).transpose(0, 2, 1, 3)


def stick_breaking_attention(q, k, v):
    seq = q.shape[2]
    scale = HEAD_DIM ** -0.5
    outs = []
    for q0 in range(0, seq, Q_BLOCK):
        kl = q0 + Q_BLOCK
        z = jnp.einsum('bhqd,bhkd->bhqk', q[:, :, q0:kl], k[:, :, :kl]).astype(jnp.float32) * scale
        t_pos = q0 + jnp.arange(Q_BLOCK)[:, None]
        s_pos = jnp.arange(kl)[None, :]
        valid = s_pos < t_pos
        log1m = jnp.where(valid, jax.nn.log_sigmoid(-z), 0.0)
        after = lax.cumsum(log1m, axis=3, reverse=True) - log1m
        w = jnp.where(valid, jnp.exp(jax.nn.log_sigmoid(z) + after), 0.0)
        outs.append(jnp.einsum('bhqk,bhkd->bhqd', w.astype(v.dtype), v[:, :, :kl]))
    return jnp.concatenate(outs, axis=2)


def differential_attention(q1, q2, k1, k2, v, lam):
    seq = q1.shape[2]
    scale = DA_HALF ** -0.5
    slopes = jnp.exp2(-8.0 * (jnp.arange(DA_HEADS, dtype=jnp.float32) + 1.0) / DA_HEADS)
    outs = []
    for q0 in range(0, seq, Q_BLOCK):
        kl = q0 + Q_BLOCK
        t_pos = q0 + jnp.arange(Q_BLOCK)[:, None]
        s_pos = jnp.arange(kl)[None, :]
        allowed = (s_pos // CHUNK) <= (t_pos // CHUNK)
        bias = -slopes[:, None, None] * jnp.abs(t_pos - s_pos).astype(jnp.float32)[None]

        def attn_map(qh, kh):
            sc = jnp.einsum('bhqd,bhkd->bhqk', qh[:, :, q0:kl], kh[:, :, :kl]).astype(jnp.float32) * scale
            return jax.nn.softmax(jnp.where(allowed, sc + bias, -jnp.inf), axis=-1)

        p = attn_map(q1, k1) - lam * attn_map(q2, k2)
        outs.append(jnp.einsum('bhqk,bhkd->bhqd', p.astype(v.dtype), v[:, :, :kl]))
    return jnp.concatenate(outs, axis=2)


def conformer_conv(a, g, conv_w, conv_b, ln_g, ln_b, w_pw):
    u = a * jax.nn.sigmoid(g)
    u = lax.conv_general_dilated(
        u, conv_w.reshape(CONV_KERNEL, 1, CONV_WIDTH).astype(u.dtype),
        window_strides=(1,), padding=[(CONV_KERNEL - 1, 0)],
        dimension_numbers=('NWC', 'WIO', 'NWC'), feature_group_count=CONV_WIDTH) + conv_b
    uf = u.astype(jnp.float32)
    mu = jnp.mean(uf, axis=-1, keepdims=True)
    var = jnp.mean(jnp.square(uf - mu), axis=-1, keepdims=True)
    un = ((uf - mu) * lax.rsqrt(var + EPS) * ln_g + ln_b).astype(u.dtype)
    return jax.nn.silu(un) @ w_pw


def swiglu(h, wg, wu, wd):
    return (jax.nn.silu(h @ wg) * (h @ wu)) @ wd


def moe_swiglu(h, w_router, e_gate, e_up, e_down):
    b, s, d = h.shape
    tok = h.reshape(b * s, d)
    logits = (tok @ w_router).astype(jnp.float32)
    top_val, top_idx = lax.top_k(logits, TOP_K)
    gates = jax.nn.softmax(top_val, axis=-1)
    combine = jnp.sum(jax.nn.one_hot(top_idx, N_EXPERTS, dtype=jnp.float32) * gates[..., None], axis=1)
    y = jnp.zeros_like(tok)
    for e in range(N_EXPERTS):
        y = y + combine[:, e:e + 1].astype(tok.dtype) * swiglu(tok, e_gate[e], e_up[e], e_down[e])
    return y.reshape(b, s, d)


def setup_inputs(seed: int = 0) -> dict:
    key = jax.random.key(seed)
    ks = jax.random.split(key, 24)
    f32 = jnp.float32
    nrm = lambda k, shape, scale: jax.random.normal(k, shape, f32) * scale
    return {
        "x": nrm(ks[0], (BATCH, SEQ, D_MODEL), 1.0),
        "attn_norm": 1.0 + nrm(ks[1], (DEPTH, D_MODEL), 0.05),
        "w_in": nrm(ks[2], (DEPTH, D_MODEL, IN_WIDTH), D_MODEL ** -0.5),
        "w_out": nrm(ks[3], (DEPTH, D_MODEL, D_MODEL), D_MODEL ** -0.5),
        "lam": nrm(ks[4], (DEPTH, 4, DA_HALF), 0.1),
        "diff_norm": 1.0 + nrm(ks[5], (DEPTH, DA_WIDTH), 0.05),
        "conv_w": nrm(ks[6], (DEPTH, CONV_KERNEL, CONV_WIDTH), CONV_KERNEL ** -0.5),
        "conv_b": nrm(ks[7], (DEPTH, CONV_WIDTH), 0.02),
        "conv_ln_g": 1.0 + nrm(ks[8], (DEPTH, CONV_WIDTH), 0.05),
        "conv_ln_b": nrm(ks[9], (DEPTH, CONV_WIDTH), 0.02),
        "w_conv_out": nrm(ks[10], (DEPTH, CONV_WIDTH, CONV_WIDTH), CONV_WIDTH ** -0.5),
        "ffn_norm": 1.0 + nrm(ks[11], (DEPTH, D_MODEL), 0.05),
        "w_gate": nrm(ks[12], (N_DENSE, D_MODEL, D_FF), D_MODEL ** -0.5),
        "w_up": nrm(ks[13], (N_DENSE, D_MODEL, D_FF), D_MODEL ** -0.5),
        "w_down": nrm(ks[14], (N_DENSE, D_FF, D_MODEL), D_FF ** -0.5),
        "w_router": nrm(ks[15], (N_MOE, D_MODEL, N_EXPERTS), D_MODEL ** -0.5),
        "e_gate": nrm(ks[16], (N_MOE, N_EXPERTS, D_MODEL, D_FF_EXPERT), D_MODEL ** -0.5),
        "e_up": nrm(ks[17], (N_MOE, N_EXPERTS, D_MODEL, D_FF_EXPERT), D_MODEL ** -0.5),
        "e_down": nrm(ks[18], (N_MOE, N_EXPERTS, D_FF_EXPERT, D_MODEL), D_FF_EXPERT ** -0.5),
        "final_norm": 1.0 + nrm(ks[19], (D_MODEL,), 0.05),
    }


def reference(x, attn_norm, w_in, w_out, lam, diff_norm, conv_w, conv_b, conv_ln_g, conv_ln_b,
              w_conv_out, ffn_norm, w_gate, w_up, w_down, w_router, e_gate, e_up, e_down, final_norm):
    b, s, _ = x.shape
    sizes = [SB_WIDTH] * 3 + [CONV_WIDTH] * 2 + [DA_WIDTH] * 3
    offs = []
    acc = 0
    for w in sizes[:-1]:
        acc += w
        offs.append(acc)
    for l in range(DEPTH):
        h = rms_norm(x, attn_norm[l])
        proj = h @ w_in[l]
        sb_q, sb_k, sb_v, glu_a, glu_g, da_q, da_k, da_v = jnp.split(proj, offs, axis=-1)

        sb_o = stick_breaking_attention(to_heads(sb_q, SB_HEADS, HEAD_DIM),
                                        to_heads(sb_k, SB_HEADS, HEAD_DIM),
                                        to_heads(sb_v, SB_HEADS, HEAD_DIM))
        sb_o = sb_o.transpose(0, 2, 1, 3).reshape(b, s, SB_WIDTH)

        cv_o = conformer_conv(glu_a, glu_g, conv_w[l], conv_b[l], conv_ln_g[l], conv_ln_b[l], w_conv_out[l])

        lam_init = 0.8 - 0.6 * math.exp(-0.3 * l)
        lv = lam[l].astype(jnp.float32)
        lam_full = jnp.exp(jnp.sum(lv[0] * lv[1])) - jnp.exp(jnp.sum(lv[2] * lv[3])) + lam_init
        qh = da_q.reshape(b, s, DA_HEADS, 2, DA_HALF).transpose(0, 2, 3, 1, 4)
        kh = da_k.reshape(b, s, DA_HEADS, 2, DA_HALF).transpose(0, 2, 3, 1, 4)
        da_o = differential_attention(qh[:, :, 0], qh[:, :, 1], kh[:, :, 0], kh[:, :, 1],
                                      to_heads(da_v, DA_HEADS, HEAD_DIM), lam_full)
        da_o = da_o.transpose(0, 2, 1, 3)
        da_o = rms_norm(da_o, diff_norm[l].reshape(DA_HEADS, HEAD_DIM)) * (1.0 - lam_init)
        da_o = da_o.reshape(b, s, DA_WIDTH).astype(x.dtype)

        x = x + jnp.concatenate([sb_o, cv_o, da_o], axis=-1) @ w_out[l]

        h = rms_norm(x, ffn_norm[l])
        i = l // 2
        if l % 2 == 0:
            x = x + swiglu(h, w_gate[i], w_up[i], w_down[i])
        else:
            x = x + moe_swiglu(h, w_router[i], e_gate[i], e_up[i], e_down[i])
    return rms_norm(x, final_norm)
```

```python
import contextlib
import numpy as np
import concourse.bass as bass
import concourse.mybir as mybir

F32 = mybir.dt.float32
BF16 = mybir.dt.bfloat16
AF = mybir.ActivationFunctionType
ALU = mybir.AluOpType
AX = mybir.AxisListType

ENGS = ["pe", "act", "dve", "pool", "sp"]


class Buf:
    def __init__(self, name, t=None):
        self.name = name
        self.t = t
        self.w = None
        self.readers = []

    def __getitem__(self, k):
        return self.t[k]


class Sched:
    def __init__(self, nc, same_engine_sync=True):
        self.nc = nc
        self.prog = {e: [] for e in ENGS}
        self.cnt = {}
        self.waited = {}
        self.same = same_engine_sync
        self.stack = contextlib.ExitStack()
        self.stacks = [self.stack]
        self.ntile = 0

    def sbuf(self, name, shape, dtype):
        self.ntile += 1
        t = self.stacks[-1].enter_context(self.nc.sbuf_tensor(f"{name}_{self.ntile}", list(shape), dtype))
        return Buf(name, t)

    @contextlib.contextmanager
    def scope(self):
        st = contextlib.ExitStack()
        self.stacks.append(st)
        try:
            yield
        finally:
            self.barrier()
            self.stacks.pop()
            st.close()

    def psum(self, name, shape, dtype=F32):
        self.ntile += 1
        t = self.stacks[-1].enter_context(self.nc.psum_tensor(f"{name}_{self.ntile}", list(shape), dtype))
        return Buf(name, t)

    def _deps(self, reads, writes):
        deps = {}
        def add(d):
            if d is None:
                return
            k, v = d
            if k.startswith("ld_c"):
                v = self.cnt[k]
            if deps.get(k, 0) < v:
                deps[k] = v
        for b in reads:
            add(b.w)
        for b in writes:
            add(b.w)
            for r in b.readers:
                add(r)
        return deps

    def op(self, eng, fn, reads=(), writes=(), dma=None, n=1, inc=None):
        deps = self._deps(reads, writes)
        for k, v in deps.items():
            if dma is None and k == eng:
                if eng == "pe" or not self.same:
                    continue
            if self.waited.get((eng, k), 0) >= v:
                continue
            self.waited[(eng, k)] = v
            self.prog[eng].append(("wait", k, v))
        key = eng if dma is None else dma
        inc = inc if inc is not None else (1 if dma is None else 16)
        self.cnt[key] = self.cnt.get(key, 0) + inc * n
        val = self.cnt[key]
        self.prog[eng].append(("op", fn, key, inc, n))
        for b in reads:
            b.readers.append((key, val))
        for b in writes:
            b.w = (key, val)
            b.readers = []
        return val

    def wait_all(self, eng, keys=None):
        for k, v in self.cnt.items():
            if keys is not None and k not in keys:
                continue
            if k == eng:
                continue
            if self.waited.get((eng, k), 0) >= v:
                continue
            self.waited[(eng, k)] = v
            self.prog[eng].append(("wait", k, v))

    def barrier(self):
        for e in ENGS:
            self.wait_all(e)

    def emit(self):
        nc = self.nc
        sems = {}
        for k in self.cnt:
            sems[k] = self.stack.enter_context(nc.semaphore(f"s_{k}"))
        prog = self.prog

        def run(e, lst):
            for item in lst:
                if item[0] == "wait":
                    e.wait_ge(sems[item[1]], item[2])
                else:
                    _, fn, key, inc, _n = item
                    r = fn(e)
                    if isinstance(r, (list, tuple)):
                        for ins in r:
                            ins.then_inc(sems[key], inc)
                    else:
                        r.then_inc(sems[key], inc)

        with nc.Block() as block:
            block.tensor(lambda e: run(e, prog["pe"]))
            block.scalar(lambda e: run(e, prog["act"]))
            block.vector(lambda e: run(e, prog["dve"]))
            block.gpsimd(lambda e: run(e, prog["pool"]))
            block.sync(lambda e: run(e, prog["sp"]))
        self.stack.close()


D = 2048
SEQ = 4096
NB = 4
TOK = 2048
EPS = 1e-6
IN_W = 5632
D_FF = 5632
D_FFE = 2816
NE = 8


def ring(lst, i):
    return lst[i % len(lst)]


def emit_norm_tile(S, C, xt, gbc, hstage, col0, ps_ring, it, extra=None):
    junk = ring(C["junk"], it)
    ss = ring(C["ss"], it)
    rstd = ring(C["rstd"], it)
    hb = ring(C["hb"], it)
    S.op("act", lambda e: e.activation(out=junk[:], in_=xt[:], func=AF.Square, accum_out=ss[:]),
         reads=[xt], writes=[junk, ss])
    S.op("act", lambda e: e.activation(out=rstd[:], in_=ss[:], func=AF.Sqrt, scale=1.0 / D, bias=C["eps"][:, 0:1]),
         reads=[ss, C["eps"]], writes=[rstd])
    S.op("dve", lambda e: e.reciprocal(out=rstd[:], in_=rstd[:]), reads=[rstd], writes=[rstd])
    if extra is not None:
        hf = extra
        S.op("dve", lambda e: e.scalar_tensor_tensor(out=hf[:], in0=xt[:], scalar=rstd[:, 0:1], in1=gbc[:],
                                                     op0=ALU.mult, op1=ALU.mult),
             reads=[xt, rstd, gbc], writes=[hf])
        S.op("pool", lambda e: e.tensor_copy(out=hb[:], in_=hf[:]), reads=[hf], writes=[hb])
    else:
        S.op("dve", lambda e: e.scalar_tensor_tensor(out=hb[:], in0=xt[:], scalar=rstd[:, 0:1], in1=gbc[:],
                                                     op0=ALU.mult, op1=ALU.mult),
             reads=[xt, rstd, gbc], writes=[hb])
    ident = C["ident_bf"]
    for half in range(2):
        ps = ring(ps_ring, it * 2 + half)
        for k in range(8):
            c = half * 8 + k
            S.op("pe", lambda e, c=c, k=k, ps=ps: e.transpose(out=ps[:, k * 128:(k + 1) * 128],
                                                               in_=hb[:, c * 128:(c + 1) * 128],
                                                               identity=ident[:]),
                 reads=[hb, ident], writes=[ps])
        eng = "act" if half == 0 else "dve"
        if eng == "act":
            S.op("act", lambda e, ps=ps, half=half: e.activation(
                out=hstage[:, half * 8:(half + 1) * 8, col0:col0 + 128],
                in_=ps[:].rearrange("p (c t) -> p c t", c=8), func=AF.Copy),
                reads=[ps], writes=[hstage])
        else:
            S.op("dve", lambda e, ps=ps, half=half: e.tensor_copy(
                out=hstage[:, half * 8:(half + 1) * 8, col0:col0 + 128],
                in_=ps[:].rearrange("p (c t) -> p c t", c=8)),
                reads=[ps], writes=[hstage])
    return rstd


def norm_ctx(S, consts):
    C = dict(consts)
    C["eps"] = S.sbuf("epsc", [128, 1], F32)
    S.op("pool", lambda e: e.memset(C["eps"][:], EPS), writes=[C["eps"]])
    C["junk"] = [S.sbuf("junk", [128, D], BF16) for _ in range(1)]
    C["ss"] = [S.sbuf("ss", [128, 1], F32) for _ in range(2)]
    C["rstd"] = [S.sbuf("rstd", [128, 1], F32) for _ in range(2)]
    C["hb"] = [S.sbuf("hb", [128, D], BF16) for _ in range(2)]
    return C


def phase_norm(S, consts, x_dram, g_dram, hT_dram):
    C = norm_ctx(S, consts)
    gbc = S.sbuf("gbc", [128, D], F32)
    S.op("sp", lambda e: e.dma_start(out=gbc[:], in_=g_dram.partition_broadcast(128)), writes=[gbc], dma="ld_c_g")
    xs = [S.sbuf("xs", [128, D], F32) for _ in range(2)]
    hst = [S.sbuf("hst", [128, 16, 512], BF16) for _ in range(2)]
    ps_ring = [S.psum("pst", [128, 1024], BF16) for _ in range(2)]
    hT_v = hT_dram.rearrange("(c p) t -> p c t", p=128)
    def ldx(tt):
        xt = ring(xs, tt)
        S.op("sp", lambda e, xt=xt, tt=tt: e.dma_start(out=xt[:], in_=x_dram[tt * 128:(tt + 1) * 128, :]),
             writes=[xt], dma=f"ld_x{tt % 2}")
    ldx(0)
    for tt in range(TOK // 128):
        xt = ring(xs, tt)
        if tt + 1 < TOK // 128:
            ldx(tt + 1)
        hstage = ring(hst, tt // 4)
        emit_norm_tile(S, C, xt, gbc, hstage, (tt % 4) * 128, ps_ring, tt)
        if tt % 4 == 3:
            g = tt // 4
            S.op("sp", lambda e, hstage=hstage, g=g: e.dma_start(out=hT_v[:, :, g * 512:(g + 1) * 512], in_=hstage[:]),
                 reads=[hstage], dma=f"st_h{g % 2}")


def load_cast(S, C, dst, dst_ap, src_ap, shape, it, eng="pool"):
    st = ring(C["wstage"], it)
    a, b = shape[1], shape[2]
    view = lambda: st[:, 0:a * b].rearrange("p (a b) -> p a b", a=a)
    S.op("sp", lambda e: e.dma_start(out=view(), in_=src_ap), writes=[st], dma=f"ld_w{it % len(C['wstage'])}")
    if eng == "act":
        S.op("act", lambda e: e.activation(out=dst_ap, in_=view(), func=AF.Copy), reads=[st], writes=[dst])
    else:
        S.op(eng, lambda e: e.tensor_copy(out=dst_ap, in_=view()), reads=[st], writes=[dst])


def evac(S, it, out_ap, out_buf, ps, ps_ap, scale=None):
    if it % 2 == 0:
        if scale is None:
            S.op("act", lambda e: e.activation(out=out_ap, in_=ps_ap, func=AF.Copy), reads=[ps], writes=[out_buf])
        else:
            S.op("act", lambda e: e.activation(out=out_ap, in_=ps_ap, func=AF.Copy, scale=scale), reads=[ps], writes=[out_buf])
    else:
        if scale is None:
            S.op("dve", lambda e: e.tensor_copy(out=out_ap, in_=ps_ap), reads=[ps], writes=[out_buf])
        else:
            S.op("dve", lambda e: e.tensor_scalar(out=out_ap, in0=ps_ap, scalar1=scale, scalar2=None, op0=ALU.mult),
                 reads=[ps], writes=[out_buf])


SB_SCALE = 128 ** -0.5
DEBUG_SKIP = set()
NQG = SEQ // 256


def proj_heads(S, C, hsrc, w_dram, pb, kscale, split_q=False):
    if split_q:
        qT = [S.sbuf("qT", [128, 2, SEQ], BF16) for _ in range(3)]
        for hl in range(3):
            S.op("pool", lambda e, hl=hl: e.memset(qT[hl][:], 0.0), writes=[qT[hl]])
    else:
        qT = [S.sbuf("qT", [128, SEQ], BF16) for _ in range(3)]
    kT = [S.sbuf("kT", [128, SEQ], BF16) for _ in range(3)]
    v = S.sbuf("v", [128, SEQ // 128, 384], BF16)
    with S.scope():
        w = S.sbuf("wproj", [128, 16, 1152], BF16)
        C2 = dict(C)
        C2["wstage"] = [S.sbuf("wstage", [128, 2048], F32) for _ in range(2)]
        for i in range(9):
            load_cast(S, C2, w, w[:, :, i * 128:(i + 1) * 128],
                      w_dram[:, i * 128:(i + 1) * 128].rearrange("(c p) n -> p c n", p=128), [128, 16, 128], i,
                      eng=["dve", "act", "pool"][i % 3])
        hts = [S.sbuf("hts", [128, 16, 256], BF16) for _ in range(2)]
        ei = 0
        pi = 0
        def ldh(tg):
            ht = ring(hts, tg)
            pieces = hsrc(tg)
            S.op("sp", lambda e, ht=ht, pieces=pieces: [e.dma_start(out=ht[:, c0:c0 + n_, :], in_=ap_) for (c0, n_, ap_) in pieces],
                 writes=[ht], dma=f"ld_h{tg % 2}", n=len(pieces))
        ldh(0)
        for tg in range(SEQ // 256):
            ht = ring(hts, tg)
            if tg + 1 < SEQ // 256:
                ldh(tg + 1)
            for which in range(2):
                for hl in range(3):
                    ps = ring(pb, pi); pi += 1
                    col = which * 384 + hl * 128
                    for c in range(16):
                        S.op("pe", lambda e, ps=ps, c=c, col=col, ht=ht: e.matmul(
                            ps[:, 0:256], lhsT=w[:, c, col:col + 128], rhs=ht[:, c, :], start=(c == 0), stop=(c == 15)),
                            reads=[w, ht], writes=[ps])
                    dst = (qT if which == 0 else kT)[hl]
                    if which == 0 and split_q:
                        evac(S, 0, dst[0:64, 0, tg * 256:(tg + 1) * 256], dst, ps, ps[0:64, 0:256])
                        evac(S, 1, dst[64:128, 1, tg * 256:(tg + 1) * 256], dst, ps, ps[64:128, 0:256])
                    else:
                        evac(S, ei, dst[:, tg * 256:(tg + 1) * 256], dst, ps, ps[:, 0:256],
                             scale=(kscale if (which == 1 and kscale is not None) else None))
                    ei += 1
            for tt in range(2):
                ps = ring(pb, pi); pi += 1
                for c in range(16):
                    S.op("pe", lambda e, ps=ps, c=c, tt=tt, ht=ht: e.matmul(
                        ps[:, 0:384], lhsT=ht[:, c, tt * 128:(tt + 1) * 128], rhs=w[:, c, 768:1152],
                        start=(c == 0), stop=(c == 15)), reads=[w, ht], writes=[ps])
                evac(S, ei, v[:, tg * 2 + tt, :], v, ps, ps[:, 0:384])
                ei += 1
    return qT, kT, v


def attn_sb(S, C, qT, kT, v, oT, pb):
    Aring = [pb[0], pb[1]]
    Bb = [pb[2], pb[3], pb[4]]
    Cc = [pb[5], pb[6], pb[7]]
    e_sb = [[S.sbuf("e", [128, 512], F32) for _ in range(2)] for _ in range(3)]
    sp_sb = [[S.sbuf("sp", [128, 512], BF16) for _ in range(3)] for _ in range(3)]
    spm_sb = [[S.sbuf("spm", [128, 512], BF16) for _ in range(3)] for _ in range(3)]
    w_sb = [[S.sbuf("w", [128, 512], BF16) for _ in range(3)] for _ in range(3)]
    wm_sb = [[S.sbuf("wm", [128, 512], BF16) for _ in range(3)] for _ in range(3)]
    R = [[S.sbuf("R", [128, 512], BF16) for _ in range(2)] for _ in range(3)]
    negtri, negones, mask = C["negtri"], C["negones"], C["sbmask"]
    tiles = []
    for gq in range(SEQ // 512):
        jlist = list(range(4 * gq + 3, -1, -1))
        for n, j in enumerate(jlist):
            tiles.append(dict(gq=gq, j=j, n=n, first=(n == 0), last=(n == len(jlist) - 1), it=len(tiles)))
    st = {}

    def stageA(t):
        it, j, gq = t["it"], t["j"], t["gq"]
        qs = slice(gq * 512, (gq + 1) * 512); ks = slice(j * 128, (j + 1) * 128)
        diag = j >= 4 * gq
        sps = {}
        for hl in range(3):
            A = ring(Aring, it * 3 + hl)
            S.op("pe", lambda e, A=A, hl=hl, ks=ks, qs=qs: e.matmul(A[:, 0:512], lhsT=kT[hl][:, ks], rhs=qT[hl][:, qs], start=True, stop=True),
                 reads=[kT[hl], qT[hl]], writes=[A])
            eb = ring(e_sb[hl], it)
            S.op("act", lambda e, A=A, eb=eb: e.activation(out=eb[:], in_=A[:, 0:512], func=AF.Exp), reads=[A], writes=[eb])
            spb = ring(sp_sb[hl], it)
            S.op("act", lambda e, eb=eb, spb=spb: e.activation(out=spb[:], in_=eb[:], func=AF.Ln, bias=C["one"][:, 0:1]),
                 reads=[eb, C["one"]], writes=[spb])
            if diag:
                spm = ring(spm_sb[hl], it)
                jj = j - 4 * gq
                S.op("pool", lambda e, spm=spm, spb=spb, jj=jj: e.tensor_tensor(out=spm[:], in0=spb[:], in1=mask[:, jj, :], op=ALU.mult),
                     reads=[spb, mask], writes=[spm])
                spb = spm
            sps[hl] = spb
        st[it] = {"sp": sps}

    def stageB(t):
        it, j, gq, n, first, last = t["it"], t["j"], t["gq"], t["n"], t["first"], t["last"]
        qs = slice(gq * 512, (gq + 1) * 512); ks = slice(j * 128, (j + 1) * 128)
        diag = j >= 4 * gq
        ws = {}
        for hl in range(3):
            B = Bb[hl]
            spb = st[it]["sp"][hl]
            Rprev = ring(R[hl], n - 1)
            Rnew = ring(R[hl], n)
            S.op("pe", lambda e, B=B, hl=hl, ks=ks, qs=qs: e.matmul(B[:, 0:512], lhsT=kT[hl][:, ks], rhs=qT[hl][:, qs], start=True, stop=False),
                 reads=[kT[hl], qT[hl]], writes=[B])
            S.op("pe", lambda e, B=B, spb=spb, first=first: e.matmul(B[:, 0:512], lhsT=negtri[:], rhs=spb[:], start=False, stop=first),
                 reads=[negtri, spb], writes=[B])
            if not first:
                S.op("pe", lambda e, B=B, Rprev=Rprev: e.matmul(B[:, 0:512], lhsT=negones[:], rhs=Rprev[:], start=False, stop=True),
                     reads=[negones, Rprev], writes=[B])
            if not last:
                if first:
                    S.op("dve", lambda e, Rnew=Rnew, spb=spb: e.tensor_copy(out=Rnew[:], in_=spb[:]), reads=[spb], writes=[Rnew])
                else:
                    S.op("dve", lambda e, Rnew=Rnew, Rprev=Rprev, spb=spb: e.tensor_tensor(out=Rnew[:], in0=Rprev[:], in1=spb[:], op=ALU.add),
                         reads=[spb, Rprev], writes=[Rnew])
            wb = ring(w_sb[hl], it)
            S.op("act", lambda e, B=B, wb=wb: e.activation(out=wb[:], in_=B[:, 0:512], func=AF.Exp), reads=[B], writes=[wb])
            if diag:
                wm = ring(wm_sb[hl], it)
                jj = j - 4 * gq
                S.op("pool", lambda e, wm=wm, wb=wb, jj=jj: e.tensor_tensor(out=wm[:], in0=wb[:], in1=mask[:, jj, :], op=ALU.mult),
                     reads=[wb, mask], writes=[wm])
                wb = wm
            ws[hl] = wb
        st[it]["w"] = ws

    def stageC(t):
        it, j, gq, first, last = t["it"], t["j"], t["gq"], t["first"], t["last"]
        qs = slice(gq * 512, (gq + 1) * 512)
        for hl in range(3):
            wb = st[it]["w"][hl]
            S.op("pe", lambda e, hl=hl, wb=wb, j=j, first=first, last=last: e.matmul(
                Cc[hl][:, 0:512], lhsT=v[:, j, hl * 128:(hl + 1) * 128], rhs=wb[:], start=first, stop=last),
                reads=[v, wb], writes=[Cc[hl]])
        if last:
            for hl in range(3):
                evac(S, gq * 3 + hl, oT[:, hl, qs], oT, Cc[hl], Cc[hl][:, 0:512])
        del st[it]

    stages = [stageA, stageB, stageC]
    for step in range(len(tiles) + len(stages) - 1):
        for si, stg in enumerate(stages):
            idx = step - si
            if 0 <= idx < len(tiles):
                stg(tiles[idx])


def attn_da(S, C, qT, kT, v, oT, pb, lam_init):
    Pr = [pb[0], pb[1]]
    O = [pb[2], pb[3], pb[4]]
    N = [pb[5], pb[6], pb[7]]
    E_sb = [[S.sbuf("E", [128, 512], BF16) for _ in range(3)] for _ in range(3)]
    Ef_sb = [[S.sbuf("Ef", [128, 512], F32) for _ in range(2)] for _ in range(3)]
    recs = [S.sbuf("rec", [128, 512], F32) for _ in range(3)]
    o12s = [S.sbuf("o12", [128, 512], F32) for _ in range(3)]
    obs = [S.sbuf("ob", [128, 256], F32) for _ in range(3)]
    osqs = [S.sbuf("osq", [128, 256], F32) for _ in range(3)]
    rss = [S.sbuf("rs", [128, 256], F32) for _ in range(3)]
    pending = []
    bias, maskF, ones_bf, onesf = C["dabias"], C["damask"], C["ones_bf"], C["onesf128"]
    neglam, gsc = C["neglam"], C["gscale"]
    tiles = []
    for gq in range(NQG):
        jlist = list(range(2 * gq + 1, -1, -1))
        for n, j in enumerate(jlist):
            tiles.append(dict(gq=gq, j=j, n=n, first=(n == 0), last=(n == len(jlist) - 1), it=len(tiles)))
    st = {}

    def stageA(t, hl):
        it, j, gq = t["it"], t["j"], t["gq"]
        qs = slice(gq * 256, (gq + 1) * 256); ks = slice(j * 128, (j + 1) * 128)
        diag = j >= 2 * gq
        cidx = j - 2 * gq - 1 + 32
        Es = st.setdefault(it, {})
        if True:
            P = ring(Pr, it * 3 + hl)
            for m in range(2):
                S.op("pe", lambda e, P=P, hl=hl, m=m, ks=ks, qs=qs: e.matmul(
                    P[:, m * 256:(m + 1) * 256], lhsT=kT[hl][:, ks], rhs=qT[hl][:, m, qs],
                    start=True, stop=True), reads=[kT[hl], qT[hl]], writes=[P])
            Eb = ring(E_sb[hl], it)
            if diag:
                Ef = ring(Ef_sb[hl], it)
                jj = j - 2 * gq
                S.op("act", lambda e, P=P, Ef=Ef, hl=hl, cidx=cidx: e.activation(
                    out=Ef[:], in_=P[:], func=AF.Exp, scale=0.125, bias=bias[:, hl, cidx:cidx + 1]),
                    reads=[P, bias], writes=[Ef])
                S.op("dve", lambda e, Ef=Ef, Eb=Eb, hl=hl, jj=jj: e.tensor_tensor(
                    out=Eb[:].rearrange("p (m t) -> p m t", m=2), in0=Ef[:].rearrange("p (m t) -> p m t", m=2),
                    in1=maskF[:, hl, jj:jj + 1, :].to_broadcast([128, 2, 256]), op=ALU.mult),
                    reads=[Ef, maskF], writes=[Eb])
            else:
                S.op("act", lambda e, P=P, Eb=Eb, hl=hl, cidx=cidx: e.activation(
                    out=Eb[:], in_=P[:], func=AF.Exp, scale=0.125, bias=bias[:, hl, cidx:cidx + 1]),
                    reads=[P, bias], writes=[Eb])
            Es[hl] = Eb

    def stageB(t, hl):
        it, j, gq, n, first, last = t["it"], t["j"], t["gq"], t["n"], t["first"], t["last"]
        qs = slice(gq * 256, (gq + 1) * 256)
        if True:
            Eb = st[it][hl]
            S.op("pe", lambda e, hl=hl, Eb=Eb, j=j, first=first, last=last: e.matmul(
                O[hl][:], lhsT=v[:, j, hl * 128:(hl + 1) * 128], rhs=Eb[:], start=first, stop=last),
                reads=[v, Eb], writes=[O[hl]])
            S.op("pe", lambda e, hl=hl, Eb=Eb, first=first, last=last: e.matmul(
                N[hl][:], lhsT=ones_bf[:], rhs=Eb[:], start=first, stop=last),
                reads=[ones_bf, Eb], writes=[N[hl]])
        if hl < 2:
            return
        del st[it]
        if n == 2 and pending:
            pending.pop()()
        if not last:
            return
        for hl in range(3):
            S.op("dve", lambda e, hl=hl: e.reciprocal(out=recs[hl][:], in_=N[hl][:]), reads=[N[hl]], writes=[recs[hl]])
            S.op("dve", lambda e, hl=hl: e.tensor_tensor(out=o12s[hl][:], in0=O[hl][:], in1=recs[hl][:], op=ALU.mult),
                 reads=[O[hl], recs[hl]], writes=[o12s[hl]])

        def rest(qs=qs, gq=gq):
            for hl in range(3):
                o12, ob, osq, rs = o12s[hl], obs[hl], osqs[hl], rss[hl]
                S.op("dve", lambda e, o12=o12, ob=ob: e.scalar_tensor_tensor(out=ob[:], in0=o12[:, 256:512], scalar=neglam[:, 0:1], in1=o12[:, 0:256],
                                                                             op0=ALU.mult, op1=ALU.add), reads=[o12, neglam], writes=[ob])
                S.op("pool", lambda e, ob=ob, osq=osq: e.tensor_tensor(out=osq[:], in0=ob[:], in1=ob[:], op=ALU.mult), reads=[ob], writes=[osq])
                P = ring(Pr, gq * 3 + hl)
                S.op("pe", lambda e, P=P, osq=osq: e.matmul(P[:, 0:256], lhsT=onesf[:], rhs=osq[:], start=True, stop=True),
                     reads=[onesf, osq], writes=[P])
                S.op("act", lambda e, P=P, rs=rs: e.activation(out=rs[:], in_=P[:, 0:256], func=AF.Sqrt, bias=C["eps"][:, 0:1]),
                     reads=[P, C["eps"]], writes=[rs])
                S.op("dve", lambda e, rs=rs: e.reciprocal(out=rs[:], in_=rs[:]), reads=[rs], writes=[rs])
                S.op("dve", lambda e, hl=hl, qs=qs, ob=ob, rs=rs: e.scalar_tensor_tensor(out=oT[:, hl, qs], in0=ob[:], scalar=gsc[:, hl:hl + 1], in1=rs[:],
                                                                                     op0=ALU.mult, op1=ALU.mult), reads=[ob, gsc, rs], writes=[oT])
        while pending:
            pending.pop()()
        pending.append(rest)

    for step in range(len(tiles) + 1):
        for hl in range(3):
            if step < len(tiles):
                stageA(tiles[step], hl)
            if step >= 1:
                stageB(tiles[step - 1], hl)
    while pending:
        pending.pop()()


def da_scalars(S, C, lam_dram, diffg_dram, pb, lam_init):
    lamt = S.sbuf("lamt", [1, 256], F32)
    S.op("sp", lambda e: e.dma_start(out=lamt[:], in_=lam_dram.rearrange("(o a) b -> o (a b)", o=1)), writes=[lamt], dma="ld_c_lam")
    prod = S.sbuf("lprod", [1, 128], F32)
    S.op("dve", lambda e: e.tensor_tensor(out=prod[:].rearrange("p (a b) -> p a b", a=2),
                                          in0=lamt[:].rearrange("p (a t b) -> p a t b", a=2, t=2)[:, :, 0, :],
                                          in1=lamt[:].rearrange("p (a t b) -> p a t b", a=2, t=2)[:, :, 1, :], op=ALU.mult),
         reads=[lamt], writes=[prod])
    sums = S.sbuf("lsum", [1, 2], F32)
    S.op("dve", lambda e: e.reduce_sum(out=sums[:], in_=prod[:].rearrange("p (a b) -> p a b", a=2), axis=AX.X),
         reads=[prod], writes=[sums])
    ex = S.sbuf("lex", [1, 2], F32)
    S.op("act", lambda e: e.activation(out=ex[:], in_=sums[:], func=AF.Exp), reads=[sums], writes=[ex])
    nl = S.sbuf("nl", [1, 1], F32)
    S.op("dve", lambda e: e.tensor_tensor(out=nl[:], in0=ex[:, 1:2], in1=ex[:, 0:1], op=ALU.subtract), reads=[ex], writes=[nl])
    S.op("dve", lambda e: e.tensor_scalar(out=nl[:], in0=nl[:], scalar1=-lam_init, scalar2=None, op0=ALU.add), reads=[nl], writes=[nl])
    onesf = C["onesf128"]
    S.op("pe", lambda e: e.matmul(pb[0][:, 0:1], lhsT=onesf[0:1, :], rhs=nl[:], start=True, stop=True),
         reads=[onesf, nl], writes=[pb[0]])
    neglam = S.sbuf("neglam", [128, 1], F32)
    S.op("act", lambda e: e.activation(out=neglam[:], in_=pb[0][:, 0:1], func=AF.Copy, scale=128.0), reads=[pb[0]], writes=[neglam])
    gsc = S.sbuf("gsc", [128, 3], F32)
    S.op("sp", lambda e: e.dma_start(out=gsc[:], in_=diffg_dram), writes=[gsc], dma="ld_c_gsc")
    S.op("dve", lambda e: e.tensor_scalar(out=gsc[:], in0=gsc[:], scalar1=1.0 - lam_init, scalar2=None, op0=ALU.mult), reads=[gsc], writes=[gsc])
    C["neglam"] = neglam
    C["gscale"] = gsc


def load_const(S, name, dram_ap, shape, dtype):
    b = S.sbuf(name, shape, dtype)
    S.op("sp", lambda e: e.dma_start(out=b[:], in_=dram_ap), writes=[b], dma="ld_c")
    return b


def lam_init_of(layer):
    import math
    return 0.8 - 0.6 * math.exp(-0.3 * layer)


def phase_heads(S, C, hsrc, w_sb, w_da, lam_dram, diffg_dram, oT_dram, layer):
    pb = [S.psum("pb", [128, 512], F32) for _ in range(8)]
    lam_init = lam_init_of(layer)
    C = dict(C)
    C["eps"] = S.sbuf("epsc", [128, 1], F32)
    S.op("pool", lambda e: e.memset(C["eps"][:], EPS), writes=[C["eps"]])
    C["one"] = S.sbuf("onec", [128, 1], F32)
    S.op("pool", lambda e: e.memset(C["one"][:], 1.0), writes=[C["one"]])
    da_scalars(S, C, lam_dram, diffg_dram, pb, lam_init)
    S.barrier()
    oview = oT_dram.rearrange("(h p) t -> p h t", p=128)
    for typ in range(2):
        with S.scope():
            oT = S.sbuf("oT", [128, 3, SEQ], BF16)
            qT, kT, v = proj_heads(S, C, hsrc, w_sb if typ == 0 else w_da, pb, SB_SCALE if typ == 0 else None, split_q=(typ == 1))
            S.barrier()
            with S.scope():
                if typ == 0 and "sb" not in DEBUG_SKIP:
                    attn_sb(S, C, qT, kT, v, oT, pb)
                elif typ == 1 and "da" not in DEBUG_SKIP:
                    attn_da(S, C, qT, kT, v, oT, pb, lam_init)
                else:
                    S.op("pool", lambda e, oT=oT: e.memset(oT[:], 0.0), writes=[oT])
            S.op("sp", lambda e, typ=typ, oT=oT: e.dma_start(out=oview[:, typ * 3:(typ + 1) * 3, :], in_=oT[:]), reads=[oT], dma="st_o")
            S.wait_all("sp")


def host_consts(r):
    import ml_dtypes
    bf = ml_dtypes.bfloat16
    s = np.arange(128)[:, None]
    t = np.arange(128)[None, :]
    c = {}
    c["ident_bf"] = np.eye(128, dtype=np.float32).astype(bf)
    c["ident_f"] = np.eye(128, dtype=np.float32)
    c["ones_bf"] = np.ones((128, 128), np.float32).astype(bf)
    c["negones"] = (-np.ones((128, 128), np.float32)).astype(bf)
    c["negtri"] = (-(s >= t).astype(np.float32)).astype(bf)
    c["onesf128"] = np.full((128, 128), 1.0 / 128, np.float32)
    c["onesf512"] = np.full((128, 128), 1.0 / 512, np.float32)
    tl = np.arange(512)[None, :]
    sbm = np.stack([((jj * 128 + s) < tl).astype(np.float32) for jj in range(4)], axis=1)
    c["sbmask"] = sbm.astype(bf)
    heads = np.arange(3) + 3 * r
    slopes = np.exp2(-8.0 * (heads.astype(np.float64) + 1.0) / 6.0)
    cc = np.arange(33) - 32
    c["dabias"] = (slopes[None, :, None] * (np.arange(128)[:, None, None] + 128.0 * cc[None, None, :])).astype(np.float32)
    allowed = ((s // 64) <= (t // 64)).astype(np.float64)
    mf = np.zeros((128, 3, 2, 256), np.float64)
    for hl in range(3):
        F = np.where(s > t, np.exp(-2.0 * slopes[hl] * (s - t)), 1.0) * allowed
        mf[:, hl, 0, 0:128] = F
        mf[:, hl, 0, 128:256] = 1.0
        mf[:, hl, 1, 0:128] = 0.0
        mf[:, hl, 1, 128:256] = F
    c["damask"] = mf.astype(np.float32)
    return c


CONST_SPECS = {
    "ident_bf": ([128, 128], BF16), "ident_f": ([128, 128], F32), "ones_bf": ([128, 128], BF16),
    "negones": ([128, 128], BF16), "negtri": ([128, 128], BF16), "onesf128": ([128, 128], F32),
    "onesf512": ([128, 128], F32), "sbmask": ([128, 4, 512], BF16), "dabias": ([128, 3, 33], F32),
    "damask": ([128, 3, 2, 256], F32),
}


def declare_consts(nc, S, names):
    C = {}
    for nme in names:
        shape, dt_ = CONST_SPECS[nme]
        ap = nc.dram_tensor(nme, shape, dt_, kind="ExternalInput").ap()
        C[nme] = load_const(S, nme, ap, shape, dt_)
    return C


def simulate(S):
    pc = {e: 0 for e in ENGS}
    sem = {}
    total = sum(len(S.prog[e]) for e in ENGS)
    done = 0
    while done < total:
        progressed = False
        for e in ENGS:
            lst = S.prog[e]
            while pc[e] < len(lst):
                it = lst[pc[e]]
                if it[0] == "wait":
                    if sem.get(it[1], 0) >= it[2]:
                        pc[e] += 1; done += 1; progressed = True
                    else:
                        break
                else:
                    sem[it[2]] = sem.get(it[2], 0) + it[3] * it[4] if len(it) > 4 else sem.get(it[2], 0) + it[3]
                    pc[e] += 1; done += 1; progressed = True
        if not progressed:
            info = {e: (pc[e], len(S.prog[e]), S.prog[e][pc[e]][:3] if pc[e] < len(S.prog[e]) else None) for e in ENGS}
            raise RuntimeError(f"DEADLOCK {info} sems={ {k: v for k, v in sem.items()} }")
    for k, v in S.cnt.items():
        assert sem.get(k, 0) == v, (k, sem.get(k, 0), v)
    return {e: len(S.prog[e]) for e in ENGS}


def phase_conv(S, C, hT_own, hT_halo, flag_dram, w_glu, cw_dram, cvec_dram, w_pw, cvT_dram):
    with S.scope():
        pb = [S.psum("pbc", [128, 512], F32) for _ in range(6)]
        C = dict(C)
        wg = S.sbuf("wglu", [128, 16, 1024], BF16)
        wpw = S.sbuf("wpw", [128, 4, 512], BF16)
        with S.scope():
            Cw = {"wstage": [S.sbuf("wstage", [128, 4096], F32) for _ in range(3)]}
            for i in range(4):
                load_cast(S, Cw, wg, wg[:, :, i * 256:(i + 1) * 256],
                          w_glu[:, i * 256:(i + 1) * 256].rearrange("(c p) n -> p c n", p=128), [128, 16, 256], i,
                          eng=["dve", "act", "pool"][i % 3])
            load_cast(S, Cw, wpw, wpw[:], w_pw.rearrange("(c p) n -> p c n", p=128), [128, 4, 512], 4, eng="act")
        cw = S.sbuf("cw", [128, 4, 31], F32)
        cvec = S.sbuf("cvec", [128, 3, 4], F32)
        flag = S.sbuf("flag", [128, 1], F32)
        S.op("sp", lambda e: [e.dma_start(out=cw[:], in_=cw_dram), e.dma_start(out=cvec[:], in_=cvec_dram),
                              e.dma_start(out=flag[:], in_=flag_dram)], writes=[cw, cvec, flag], dma="ld_c_cv", n=3)
        u = S.sbuf("u", [128, 4, 32 + TOK], BF16)
        dgm = S.sbuf("dgm", [128, 4, 31, 128], BF16)
        identb = C["ident_bf"]
        for cc in range(4):
            for k in range(31):
                S.op("dve", lambda e, cc=cc, k=k: e.tensor_scalar(out=dgm[:, cc, k, :], in0=identb[:], scalar1=cw[:, cc, k:k + 1], scalar2=None, op0=ALU.mult), reads=[identb, cw], writes=[dgm])
        y = S.sbuf("y", [128, 4, TOK], F32)
        hv = hT_own.rearrange("(c p) t -> p c t", p=128)
        halo_pieces = hT_halo if isinstance(hT_halo, list) else [(0, 16, hT_halo)]
        halo_pieces = [(c0, n_, ap_.rearrange("(c p) t -> p c t", p=128)) for (c0, n_, ap_) in halo_pieces]
        hts = [S.sbuf("htc", [128, 16, 512], BF16) for _ in range(2)]
        sgs = [S.sbuf("sg", [128, 512], F32) for _ in range(2)]
        pi = 0
        def ldc(grp):
            ht = ring(hts, grp)
            if grp == 0:
                S.op("sp", lambda e, ht=ht: [e.dma_start(out=ht[:, c0:c0 + n_, 0:32], in_=ap_) for (c0, n_, ap_) in halo_pieces],
                     writes=[ht], dma=f"ld_hc{grp % 2}", n=len(halo_pieces))
            else:
                S.op("sp", lambda e, ht=ht, grp=grp: e.dma_start(out=ht[:], in_=hv[:, :, (grp - 1) * 512:grp * 512]),
                     writes=[ht], dma=f"ld_hc{grp % 2}")
        ldc(0)
        for grp in range(5):
            ht = ring(hts, grp)
            if grp + 1 < 5:
                ldc(grp + 1)
            if grp == 0:
                N, off = 32, 0
            else:
                N, off = 512, 32 + (grp - 1) * 512
            for cc in range(4):
                Pa = ring(pb, pi); Pg = ring(pb, pi + 1); pi += 2
                for which, P in ((0, Pa), (1, Pg)):
                    col = which * 512 + cc * 128
                    for c in range(16):
                        S.op("pe", lambda e, P=P, c=c, col=col, ht=ht, N=N: e.matmul(
                            P[:, 0:N], lhsT=wg[:, c, col:col + 128], rhs=ht[:, c, 0:N], start=(c == 0), stop=(c == 15)),
                            reads=[wg, ht], writes=[P])
                sg = ring(sgs, pi // 2)
                S.op("act", lambda e, Pg=Pg, sg=sg, N=N: e.activation(out=sg[:, 0:N], in_=Pg[:, 0:N], func=AF.Sigmoid),
                     reads=[Pg], writes=[sg])
                S.op("dve", lambda e, Pa=Pa, sg=sg, N=N, off=off, cc=cc: e.tensor_tensor(
                    out=u[:, cc, off:off + N], in0=Pa[:, 0:N], in1=sg[:, 0:N], op=ALU.mult), reads=[Pa, sg], writes=[u])
                if grp == 0:
                    S.op("dve", lambda e, cc=cc: e.tensor_scalar(out=u[:, cc, 0:32], in0=u[:, cc, 0:32], scalar1=flag[:, 0:1],
                                                                 scalar2=None, op0=ALU.mult), reads=[u, flag], writes=[u])
        for cc in range(4):
            for tg in range(4):
                Pc = ring(pb, pi); pi += 1
                for k in range(31):
                    S.op("pe", lambda e, Pc=Pc, cc=cc, k=k, tg=tg: e.matmul(
                        Pc[:], lhsT=dgm[:, cc, k, :], rhs=u[:, cc, tg * 512 + 2 + k:tg * 512 + 2 + k + 512], start=(k == 0), stop=(k == 30)),
                        reads=[dgm, u], writes=[Pc])
                S.op("act", lambda e, Pc=Pc, cc=cc, tg=tg: e.activation(out=y[:, cc, tg * 512:(tg + 1) * 512], in_=Pc[:], func=AF.Identity,
                                                                       bias=cvec[:, 0, cc:cc + 1]), reads=[Pc, cvec], writes=[y])
        onesf = C["onesf512"]
        ysq = [S.sbuf("ysq", [128, 512], F32) for _ in range(2)]
        mean = S.sbuf("mean", [128, 512], F32)
        msq = S.sbuf("msq", [128, 512], F32)
        rstd = S.sbuf("rstdc", [128, 512], F32)
        t1 = [S.sbuf("t1", [128, 512], F32) for _ in range(2)]
        sT = S.sbuf("sT", [128, 4, 512], BF16)
        cvT = S.sbuf("cvT", [128, 4, TOK], BF16)
        for tg in range(4):
            ts_ = slice(tg * 512, (tg + 1) * 512)
            Pm = ring(pb, pi); Pq = ring(pb, pi + 1); pi += 2
            for cc in range(4):
                S.op("pe", lambda e, Pm=Pm, cc=cc, ts_=ts_: e.matmul(Pm[:], lhsT=onesf[:], rhs=y[:, cc, ts_], start=(cc == 0), stop=(cc == 3)),
                     reads=[onesf, y], writes=[Pm])
            for cc in range(4):
                yq = ring(ysq, cc)
                S.op("act", lambda e, yq=yq, cc=cc, ts_=ts_: e.activation(out=yq[:], in_=y[:, cc, ts_], func=AF.Square), reads=[y], writes=[yq])
                S.op("pe", lambda e, Pq=Pq, yq=yq, cc=cc: e.matmul(Pq[:], lhsT=onesf[:], rhs=yq[:], start=(cc == 0), stop=(cc == 3)),
                     reads=[onesf, yq], writes=[Pq])
            S.op("dve", lambda e, Pm=Pm: e.tensor_copy(out=mean[:], in_=Pm[:]), reads=[Pm], writes=[mean])
            S.op("dve", lambda e: e.tensor_tensor(out=msq[:], in0=mean[:], in1=mean[:], op=ALU.mult), reads=[mean], writes=[msq])
            S.op("dve", lambda e, Pq=Pq: e.tensor_tensor(out=msq[:], in0=Pq[:], in1=msq[:], op=ALU.subtract), reads=[Pq, msq], writes=[msq])
            S.op("act", lambda e: e.activation(out=rstd[:], in_=msq[:], func=AF.Sqrt, bias=C["eps"][:, 0:1]), reads=[msq, C["eps"]], writes=[rstd])
            S.op("dve", lambda e: e.reciprocal(out=rstd[:], in_=rstd[:]), reads=[rstd], writes=[rstd])
            for cc in range(4):
                tt1 = ring(t1, cc)
                S.op("dve", lambda e, tt1=tt1, cc=cc, ts_=ts_: e.tensor_tensor(out=tt1[:], in0=y[:, cc, ts_], in1=mean[:], op=ALU.subtract),
                     reads=[y, mean], writes=[tt1])
                S.op("dve", lambda e, tt1=tt1: e.tensor_tensor(out=tt1[:], in0=tt1[:], in1=rstd[:], op=ALU.mult), reads=[tt1, rstd], writes=[tt1])
                S.op("act", lambda e, tt1=tt1, cc=cc: e.activation(out=sT[:, cc, :], in_=tt1[:], func=AF.Silu,
                                                                 scale=cvec[:, 1, cc:cc + 1], bias=cvec[:, 2, cc:cc + 1]),
                     reads=[tt1, cvec], writes=[sT])
            for co in range(4):
                Pc = ring(pb, pi); pi += 1
                for ci in range(4):
                    S.op("pe", lambda e, Pc=Pc, ci=ci, co=co: e.matmul(Pc[:], lhsT=wpw[:, ci, co * 128:(co + 1) * 128], rhs=sT[:, ci, :],
                                                                     start=(ci == 0), stop=(ci == 3)), reads=[wpw, sT], writes=[Pc])
                evac(S, co, cvT[:, co, ts_], cvT, Pc, Pc[:])
        S.op("sp", lambda e: e.dma_start(out=cvT_dram.rearrange("(c p) t -> p c t", p=128), in_=cvT[:]), reads=[cvT], dma="st_cv")
        S.wait_all("sp")


def phase_outproj(S, C, x_dram, oT_attn, cvT_dram, w_out, g2_dram, x1_dram, h2T_dram, wr_dram=None, comb_dram=None, oload=None, omap=None):
    moe = wr_dram is not None
    with S.scope():
        pb = [S.psum("pbo", [128, 512], F32) for _ in range(4)]
        pst = [S.psum("pst", [128, 1024], BF16) for _ in range(2)]
        pf = [S.psum("pbf", [128, 512], F32) for _ in range(2)] if moe else None
        Cn = norm_ctx(S, C)
        wo = S.sbuf("wo", [128, 16, D], BF16)
        with S.scope():
            Cw = {"wstage": [S.sbuf("wstage", [128, 4096], F32) for _ in range(3)]}
            for i in range(8):
                load_cast(S, Cw, wo, wo[:, :, i * 256:(i + 1) * 256],
                          w_out[:, i * 256:(i + 1) * 256].rearrange("(c p) n -> p c n", p=128), [128, 16, 256], i,
                          eng=["dve", "act", "pool"][i % 3])
        gbc = S.sbuf("gbc", [128, D], F32)
        S.op("sp", lambda e: e.dma_start(out=gbc[:], in_=g2_dram.partition_broadcast(128)), writes=[gbc], dma="ld_c_g")
        if moe:
            wr = S.sbuf("wr", [128, 16, NE], F32)
            S.op("sp", lambda e: e.dma_start(out=wr[:], in_=wr_dram.rearrange("(c p) n -> p c n", p=128)), writes=[wr], dma="ld_c_wr")
            comb = S.sbuf("comb", [128, TOK // 128, NE], F32)
            hf = S.sbuf("hf", [128, D], F32)
            hTf = S.sbuf("hTf", [128, 16, 128], F32)
            lg = S.sbuf("lg", [128, NE], F32)
            top8 = S.sbuf("top8", [128, 8], F32)
            sc = S.sbuf("rsc", [128, 8], F32)
            m1t = S.sbuf("m1t", [128, NE], F32)
            m2t = S.sbuf("m2t", [128, NE], F32)
        xs = [S.sbuf("xs", [128, D], F32) for _ in range(2)]
        ots = [S.sbuf("ots", [128, 16, 512], BF16) for _ in range(2)]
        hst = [S.sbuf("hst", [128, 16, 512], BF16) for _ in range(2)]
        ov = oT_attn.rearrange("(c p) t -> p c t", p=128) if oT_attn is not None else None
        cv = cvT_dram.rearrange("(c p) t -> p c t", p=128)
        hT_v = h2T_dram.rearrange("(c p) t -> p c t", p=128)
        pi = 0
        def ldo(g):
            ot = ring(ots, g)
            gs = slice(g * 512, (g + 1) * 512)
            if oload is not None:
                S.op("pool", lambda e, ot=ot, g=g, gs=gs: oload(e, ot, g) + [e.dma_start(out=ot[:, 6:10, :], in_=cv[:, :, gs])],
                     writes=[ot], dma=f"ld_o{g % 2}", n=13)
                return
            mp = omap if omap is not None else [(0, 6, 0), (10, 6, 6)]
            S.op("sp", lambda e, ot=ot, gs=gs: [e.dma_start(out=ot[:, c0:c0 + n_, :], in_=ov[:, s0:s0 + n_, gs]) for (c0, n_, s0) in mp]
                 + [e.dma_start(out=ot[:, 6:10, :], in_=cv[:, :, gs])],
                 writes=[ot], dma=f"ld_o{g % 2}", n=len(mp) + 1)
        def ldx(tt):
            xt = ring(xs, tt)
            S.op("sp", lambda e, xt=xt, tt=tt: e.dma_start(out=xt[:], in_=x_dram[tt * 128:(tt + 1) * 128, :]),
                 writes=[xt], dma=f"ld_x{tt % 2}")
        ldo(0); ldx(0)

        def stageA(tt):
            nonlocal pi
            g = tt // 4
            ot = ring(ots, g)
            if tt % 4 == 0 and g + 1 < 4:
                ldo(g + 1)
            xt = ring(xs, tt)
            tl = slice((tt % 4) * 128, (tt % 4 + 1) * 128)
            for cg in range(4):
                P = ring(pb, pi); pi += 1
                for c in range(16):
                    S.op("pe", lambda e, P=P, c=c, cg=cg, ot=ot, tl=tl: e.matmul(
                        P[:], lhsT=ot[:, c, tl], rhs=wo[:, c, cg * 512:(cg + 1) * 512], start=(c == 0), stop=(c == 15)),
                        reads=[ot, wo], writes=[P])
                S.op("dve", lambda e, P=P, xt=xt, cg=cg: e.tensor_tensor(out=xt[:, cg * 512:(cg + 1) * 512], in0=P[:],
                                                                        in1=xt[:, cg * 512:(cg + 1) * 512], op=ALU.add),
                     reads=[P, xt], writes=[xt])

        def stageB(tt):
            nonlocal pi
            g = tt // 4
            xt = ring(xs, tt)
            S.op("sp", lambda e, xt=xt, tt=tt: e.dma_start(out=x1_dram[tt * 128:(tt + 1) * 128, :], in_=xt[:]),
                 reads=[xt], dma=f"st_x{tt % 2}")
            hstage = ring(hst, g)
            emit_norm_tile(S, Cn, xt, gbc, hstage, (tt % 4) * 128, pst, tt, extra=(hf if moe else None))
            if tt % 4 == 3:
                S.op("sp", lambda e, hstage=hstage, g=g: e.dma_start(out=hT_v[:, :, g * 512:(g + 1) * 512], in_=hstage[:]),
                     reads=[hstage], dma=f"st_h{g % 2}")
            if moe:
                identf = C["ident_f"]
                for q4 in range(4):
                    Pf = ring(pf, q4)
                    for k in range(4):
                        c = q4 * 4 + k
                        S.op("pe", lambda e, Pf=Pf, c=c, k=k: e.transpose(out=Pf[:, k * 128:(k + 1) * 128], in_=hf[:, c * 128:(c + 1) * 128],
                                                                           identity=identf[:]), reads=[hf, identf], writes=[Pf])
                    evac(S, q4, hTf[:, q4 * 4:(q4 + 1) * 4, :], hTf, Pf, Pf[:].rearrange("p (c t) -> p c t", c=4))
                Pl = ring(pb, pi); pi += 1
                for c in range(16):
                    S.op("pe", lambda e, Pl=Pl, c=c: e.matmul(Pl[:, 0:NE], lhsT=hTf[:, c, :], rhs=wr[:, c, :], start=(c == 0), stop=(c == 15)),
                         reads=[hTf, wr], writes=[Pl])
                S.op("dve", lambda e, Pl=Pl: e.tensor_copy(out=lg[:], in_=Pl[:, 0:NE]), reads=[Pl], writes=[lg])
                S.op("dve", lambda e: e.max(out=top8[:], in_=lg[:]), reads=[lg], writes=[top8])
                S.op("dve", lambda e: e.tensor_tensor(out=sc[:, 0:1], in0=top8[:, 1:2], in1=top8[:, 0:1], op=ALU.subtract), reads=[top8], writes=[sc])
                S.op("act", lambda e: e.activation(out=sc[:, 1:2], in_=sc[:, 0:1], func=AF.Exp), reads=[sc], writes=[sc])
                S.op("dve", lambda e: e.tensor_scalar(out=sc[:, 2:3], in0=sc[:, 1:2], scalar1=1.0, scalar2=None, op0=ALU.add), reads=[sc], writes=[sc])
                S.op("dve", lambda e: e.reciprocal(out=sc[:, 2:3], in_=sc[:, 2:3]), reads=[sc], writes=[sc])
                S.op("dve", lambda e: e.tensor_tensor(out=sc[:, 3:4], in0=sc[:, 1:2], in1=sc[:, 2:3], op=ALU.mult), reads=[sc], writes=[sc])
                S.op("dve", lambda e: e.tensor_scalar(out=m1t[:], in0=lg[:], scalar1=top8[:, 0:1], scalar2=sc[:, 2:3],
                                                      op0=ALU.is_equal, op1=ALU.mult), reads=[lg, top8, sc], writes=[m1t])
                S.op("dve", lambda e: e.tensor_scalar(out=m2t[:], in0=lg[:], scalar1=top8[:, 1:2], scalar2=sc[:, 3:4],
                                                      op0=ALU.is_equal, op1=ALU.mult), reads=[lg, top8, sc], writes=[m2t])
                S.op("dve", lambda e, tt=tt: e.tensor_tensor(out=comb[:, tt, :], in0=m1t[:], in1=m2t[:], op=ALU.add), reads=[m1t, m2t], writes=[comb])

        NT_ = TOK // 128
        for tt in range(NT_):
            stageA(tt)
            if tt >= 1:
                stageB(tt - 1)
            if tt + 1 < NT_:
                ldx(tt + 1)
        stageB(NT_ - 1)
        if moe:
            S.op("sp", lambda e: e.dma_start(out=comb_dram.rearrange("(t p) n -> p t n", p=128), in_=comb[:]), reads=[comb], dma="st_cb")
        S.wait_all("sp")


FE = 2816
NFC = FE // 128
NFB = FE // 256


def phase_ffn(S, C, h2T_dram, x1_dram, x2_dram, experts, comb_dram, aT_scr):
    with S.scope():
        pg = [S.psum("pg", [128, 512], F32) for _ in range(2)]
        pu = [S.psum("pu", [128, 512], F32) for _ in range(2)]
        py = [S.psum("py", [128, 512], F32) for _ in range(2)]
        C = dict(C)
        C["wstage"] = [S.sbuf("wstage", [128, 4096], F32) for _ in range(2)]
        h2T = S.sbuf("h2T", [128, 16, TOK], BF16)
        hv = h2T_dram.rearrange("(c p) t -> p c t", p=128)
        for g in range(4):
            S.op("sp", lambda e, g=g: e.dma_start(out=h2T[:, :, g * 512:(g + 1) * 512], in_=hv[:, :, g * 512:(g + 1) * 512]),
                 writes=[h2T], dma="ld_c_h2")
        use_comb = any(ex[3] is not None for ex in experts)
        if use_comb:
            comb = S.sbuf("combf", [128, TOK // 128, NE], F32)
            S.op("sp", lambda e: e.dma_start(out=comb[:], in_=comb_dram.rearrange("(t p) n -> p t n", p=128)), writes=[comb], dma="ld_c_cb")
        wgs = [S.sbuf("wgs", [128, 16, 128], BF16) for _ in range(2)]
        wus = [S.sbuf("wus", [128, 16, 128], BF16) for _ in range(2)]
        wds = [S.sbuf("wds", [128, NFC, 512], BF16) for _ in range(2)]
        sgs = [S.sbuf("sgf", [128, 512], F32) for _ in range(2)]
        ast = [S.sbuf("ast", [128, TOK], BF16) for _ in range(2)]
        ats = [S.sbuf("ats", [128, NFC, 128], BF16) for _ in range(3)]
        accs = [S.sbuf("acc", [128, 512], F32) for _ in range(3)]
        li = 0
        pi = 0
        NCG = D // 512
        jobs = [(cg, tt) for cg in range(NCG) for tt in range(TOK // 128)]
        for ei_, (wg_ap, wu_ap, wd_ap, ccol) in enumerate(experts):
            src_x = x1_dram if ei_ == 0 else x2_dram

            def ldw(fb, li, wg_ap=wg_ap, wu_ap=wu_ap):
                if fb >= NFC:
                    if ei_ + 1 >= len(experts):
                        return
                    wg_ap, wu_ap = experts[ei_ + 1][0], experts[ei_ + 1][1]
                    fb = 0
                wgb = ring(wgs, fb); wub = ring(wus, fb)
                fs = slice(fb * 128, (fb + 1) * 128)
                load_cast(S, C, wgb, wgb[:], wg_ap[:, fs].rearrange("(c p) n -> p c n", p=128), [128, 16, 128], li)
                load_cast(S, C, wub, wub[:], wu_ap[:, fs].rearrange("(c p) n -> p c n", p=128), [128, 16, 128], li + 1)

            def ldwd(cg, li, wd_ap=wd_ap):
                wdb = ring(wds, cg)
                cs = slice(cg * 512, (cg + 1) * 512)
                for k, (f0, nf) in enumerate(((0, 8), (8, 8), (16, 6))):
                    load_cast(S, C, wdb, wdb[:, f0:f0 + nf, :],
                              wd_ap[f0 * 128:(f0 + nf) * 128, cs].rearrange("(f p) n -> p f n", p=128), [128, nf, 512], li + k)

            if ei_ > 0:
                S.wait_all("sp", keys=[k for k in S.cnt if k.startswith("ld_a")])
            if ei_ == 0:
                ldw(0, li); li += 2
            for fb in range(NFC):
                wgb = ring(wgs, fb); wub = ring(wus, fb)
                if fb + 1 < NFC:
                    ldw(fb + 1, li); li += 2
                if fb == NFC - 2:
                    ldwd(0, li); li += 3
                a_st = ring(ast, fb)
                for tg in range(4):
                    G = ring(pg, pi); U = ring(pu, pi); pi += 1
                    ts_ = slice(tg * 512, (tg + 1) * 512)
                    for c in range(16):
                        S.op("pe", lambda e, G=G, c=c, wgb=wgb, ts_=ts_: e.matmul(
                            G[:], lhsT=wgb[:, c, :], rhs=h2T[:, c, ts_], start=(c == 0), stop=(c == 15)),
                            reads=[wgb, h2T], writes=[G])
                    for c in range(16):
                        S.op("pe", lambda e, U=U, c=c, wub=wub, ts_=ts_: e.matmul(
                            U[:], lhsT=wub[:, c, :], rhs=h2T[:, c, ts_], start=(c == 0), stop=(c == 15)),
                            reads=[wub, h2T], writes=[U])
                    sg = ring(sgs, pi)
                    S.op("act", lambda e, G=G, sg=sg: e.activation(out=sg[:], in_=G[:], func=AF.Silu), reads=[G], writes=[sg])
                    S.op("dve", lambda e, U=U, sg=sg, a_st=a_st, ts_=ts_: e.tensor_tensor(
                        out=a_st[:, ts_], in0=U[:], in1=sg[:], op=ALU.mult), reads=[U, sg], writes=[a_st])
                S.op("sp", lambda e, a_st=a_st, fb=fb: e.dma_start(
                    out=aT_scr[:, :, fb, :].rearrange("t p k -> p t k"),
                    in_=a_st[:].rearrange("p (t k) -> p t k", k=128)), reads=[a_st], dma=f"st_a{fb % 2}")
            S.wait_all("sp", keys=[k for k in S.cnt if k.startswith("st_a") or k.startswith("st_y")])

            def ldat(i):
                cg, tt = jobs[i]
                at = ring(ats, i)
                S.op("sp", lambda e, at=at, tt=tt: e.dma_start(out=at[:], in_=aT_scr[tt]), writes=[at], dma=f"ld_a{i % 3}")

            def ldacc(i, src_x=src_x):
                cg, tt = jobs[i]
                cs = slice(cg * 512, (cg + 1) * 512)
                acc = ring(accs, i)
                S.op("sp", lambda e, acc=acc, tt=tt, cs=cs, src_x=src_x: e.dma_start(out=acc[:], in_=src_x[tt * 128:(tt + 1) * 128, cs]),
                     writes=[acc], dma=f"ld_y{i % 3}")
            ldat(0); ldat(1); ldacc(0); ldacc(1)
            for i, (cg, tt) in enumerate(jobs):
                wdb = ring(wds, cg)
                cs = slice(cg * 512, (cg + 1) * 512)
                if tt == 0 and cg + 1 < NCG:
                    ldwd(cg + 1, li); li += 3
                if i == len(jobs) - 6:
                    ldw(NFC, li); li += 2
                if i + 2 < len(jobs):
                    ldat(i + 2)
                if i + 2 < len(jobs):
                    ldacc(i + 2)
                at = ring(ats, i); acc = ring(accs, i)
                Y = ring(py, i)
                for f in range(NFC):
                    S.op("pe", lambda e, Y=Y, f=f, at=at, wdb=wdb: e.matmul(Y[:], lhsT=at[:, f, :], rhs=wdb[:, f, :],
                                                                           start=(f == 0), stop=(f == NFC - 1)),
                         reads=[at, wdb], writes=[Y])
                if ccol is None:
                    S.op("dve", lambda e, Y=Y, acc=acc: e.tensor_tensor(out=acc[:], in0=Y[:], in1=acc[:], op=ALU.add),
                         reads=[Y, acc], writes=[acc])
                else:
                    S.op("dve", lambda e, Y=Y, acc=acc, tt=tt, ccol=ccol: e.scalar_tensor_tensor(
                        out=acc[:], in0=Y[:], scalar=comb[:, tt, ccol:ccol + 1], in1=acc[:], op0=ALU.mult, op1=ALU.add),
                        reads=[Y, acc, comb], writes=[acc])
                S.op("sp", lambda e, acc=acc, tt=tt, cs=cs: e.dma_start(out=x2_dram[tt * 128:(tt + 1) * 128, cs], in_=acc[:]),
                     reads=[acc], dma=f"st_y{i % 3}")
        S.barrier()


def phase_final_norm(S, C, x_dram, g_dram, out_dram):
    with S.scope():
        Cn = norm_ctx(S, C)
        gbc = S.sbuf("gbc", [128, D], F32)
        S.op("sp", lambda e: e.dma_start(out=gbc[:], in_=g_dram.partition_broadcast(128)), writes=[gbc], dma="ld_c_g")
        xs = [S.sbuf("xs", [128, D], F32) for _ in range(2)]
        os_ = [S.sbuf("os", [128, D], F32) for _ in range(2)]
        for tt in range(TOK // 128):
            xt = ring(xs, tt); ot = ring(os_, tt)
            junk = Cn["junk"][0]; ss = ring(Cn["ss"], tt); rstd = ring(Cn["rstd"], tt)
            S.op("sp", lambda e, xt=xt, tt=tt: e.dma_start(out=xt[:], in_=x_dram[tt * 128:(tt + 1) * 128, :]), writes=[xt], dma=f"ld_x{tt % 2}")
            S.op("act", lambda e, xt=xt, ss=ss: e.activation(out=junk[:], in_=xt[:], func=AF.Square, accum_out=ss[:]), reads=[xt], writes=[junk, ss])
            S.op("act", lambda e, ss=ss, rstd=rstd: e.activation(out=rstd[:], in_=ss[:], func=AF.Sqrt, scale=1.0 / D, bias=Cn["eps"][:, 0:1]),
                 reads=[ss, Cn["eps"]], writes=[rstd])
            S.op("dve", lambda e, rstd=rstd: e.reciprocal(out=rstd[:], in_=rstd[:]), reads=[rstd], writes=[rstd])
            S.op("dve", lambda e, xt=xt, ot=ot, rstd=rstd: e.scalar_tensor_tensor(out=ot[:], in0=xt[:], scalar=rstd[:, 0:1], in1=gbc[:],
                                                                              op0=ALU.mult, op1=ALU.mult), reads=[xt, rstd, gbc], writes=[ot])
            S.op("sp", lambda e, ot=ot, tt=tt: e.dma_start(out=out_dram[tt * 128:(tt + 1) * 128, :], in_=ot[:]), reads=[ot], dma=f"st_f{tt % 2}")
        S.wait_all("sp")


from concourse.bass_utils import run_bass_kernel_spmd


def _ext(nc, name, shape, dt_, out=False):
    return nc.dram_tensor(name, list(shape), dt_, kind="ExternalOutput" if out else "ExternalInput").ap()


DEBUG_EXT = False


def _int(nc, name, shape, dt_):
    return nc.dram_tensor(name, list(shape), dt_, kind="ExternalOutput" if DEBUG_EXT else "Internal").ap()


def build_N():
    nc = bass.Bass("TRN2", target_bir_lowering=False)
    x = _ext(nc, "x", [TOK, D], F32)
    g = _ext(nc, "g", [D], F32)
    hT = _ext(nc, "hT", [D, TOK], BF16, out=True)
    S = Sched(nc)
    C = declare_consts(nc, S, ["ident_bf"])
    phase_norm(S, C, x, g, hT)
    S.barrier()
    S.emit()
    return nc


def build_H(layer):
    nc = bass.Bass("TRN2", target_bir_lowering=False)
    hT = _ext(nc, "hT_full", [D, SEQ], BF16)
    w_sb = _ext(nc, "w_sb", [D, 1152], F32)
    w_da = _ext(nc, "w_da", [D, 1152], F32)
    lam = _ext(nc, "lam", [4, 64], F32)
    dg = _ext(nc, "diffg", [128, 3], F32)
    oT = _ext(nc, "oT", [768, SEQ], BF16, out=True)
    S = Sched(nc)
    C = declare_consts(nc, S, ["ident_bf", "ones_bf", "negones", "negtri", "onesf128", "sbmask", "dabias", "damask"])
    hv = hT.rearrange("(c p) t -> p c t", p=128)
    phase_heads(S, C, lambda tg: [(0, 16, hv[:, :, tg * 256:(tg + 1) * 256])], w_sb, w_da, lam, dg, oT, layer)
    S.barrier()
    S.emit()
    return nc


def build_T(layer, last):
    moe = (layer % 2 == 1)
    nc = bass.Bass("TRN2", target_bir_lowering=False)
    x = _ext(nc, "x", [TOK, D], F32)
    hT_own = _ext(nc, "hT_own", [D, TOK], BF16)
    hT_halo = _ext(nc, "hT_halo", [D, 32], BF16)
    flag = _ext(nc, "flag", [128, 1], F32)
    oT_attn = _ext(nc, "oT_attn", [1536, TOK], BF16)
    w_glu = _ext(nc, "w_glu", [D, 1024], F32)
    cw = _ext(nc, "cw", [128, 4, 31], F32)
    cvec = _ext(nc, "cvec", [128, 3, 4], F32)
    w_pw = _ext(nc, "w_pw", [512, 512], F32)
    w_out = _ext(nc, "w_out", [D, D], F32)
    g2 = _ext(nc, "g2", [D], F32)
    gn = _ext(nc, "g_next", [D], F32)
    if moe:
        wr = _ext(nc, "w_router", [D, NE], F32)
        eg = _ext(nc, "e_gate", [NE, D, FE], F32)
        eu = _ext(nc, "e_up", [NE, D, FE], F32)
        ed = _ext(nc, "e_down", [NE, FE, D], F32)
        experts = [(eg[e], eu[e], ed[e], e) for e in range(NE)]
    else:
        wgt = _ext(nc, "w_gate", [D, D_FF], F32)
        wup = _ext(nc, "w_up", [D, D_FF], F32)
        wdn = _ext(nc, "w_down", [D_FF, D], F32)
        experts = [(wgt[:, h * FE:(h + 1) * FE], wup[:, h * FE:(h + 1) * FE], wdn[h * FE:(h + 1) * FE, :], None) for h in range(2)]
    cvT = _int(nc, "cvT", [512, TOK], BF16)
    x1 = _int(nc, "x1", [TOK, D], F32)
    h2T = _int(nc, "h2T", [D, TOK], BF16)
    comb = _int(nc, "comb", [TOK, NE], F32)
    aT_scr = _int(nc, "aT_scr", [16, 128, NFC, 128], BF16)
    if last:
        x2 = _int(nc, "x2", [TOK, D], F32)
        out = _ext(nc, "out", [TOK, D], F32, out=True)
    else:
        x2 = _ext(nc, "x2", [TOK, D], F32, out=True)
        hT_next = _ext(nc, "hT_next", [D, TOK], BF16, out=True)
    S = Sched(nc)
    C = declare_consts(nc, S, ["ident_bf", "ident_f", "onesf512"])
    C["eps"] = S.sbuf("epsc", [128, 1], F32)
    S.op("pool", lambda e: e.memset(C["eps"][:], EPS), writes=[C["eps"]])
    S.barrier()
    phase_conv(S, C, hT_own, hT_halo, flag, w_glu, cw, cvec, w_pw, cvT)
    S.barrier()
    phase_outproj(S, C, x, oT_attn, cvT, w_out, g2, x1, h2T, wr_dram=(wr if moe else None), comb_dram=(comb if moe else None))
    S.barrier()
    if "ffn" not in DEBUG_SKIP:
        phase_ffn(S, C, h2T, x1, x2, experts, comb, aT_scr)
    S.barrier()
    if last:
        phase_final_norm(S, C, x2, gn, out)
    else:
        with S.scope():
            phase_norm(S, C, x2, gn, hT_next)
    S.barrier()
    S.emit()
    return nc


RG_PAIR = [[0, 1], [2, 3], [4, 5], [6, 7]]
DEPTH = 2
H_CONSTS = ["ident_bf", "ident_f", "ones_bf", "negones", "negtri", "onesf128", "onesf512", "sbmask", "dabias", "damask"]


def build_fused():
    nc = bass.Bass("TRN2", target_bir_lowering=False)
    x_in = _ext(nc, "x", [TOK, D], F32)
    out = _ext(nc, "out", [TOK, D], F32, out=True)
    flag = _ext(nc, "flag", [128, 1], F32)
    g_attn = [_ext(nc, f"g_attn{l}", [D], F32) for l in range(DEPTH)]
    g_ffn = [_ext(nc, f"g_ffn{l}", [D], F32) for l in range(DEPTH)]
    g_fin = _ext(nc, "g_final", [D], F32)
    w_sb = [_ext(nc, f"w_sb{l}", [D, 1152], F32) for l in range(DEPTH)]
    w_da = [_ext(nc, f"w_da{l}", [D, 1152], F32) for l in range(DEPTH)]
    lam = [_ext(nc, f"lam{l}", [4, 64], F32) for l in range(DEPTH)]
    dg = [_ext(nc, f"diffg{l}", [128, 3], F32) for l in range(DEPTH)]
    w_glu = [_ext(nc, f"w_glu{l}", [D, 1024], F32) for l in range(DEPTH)]
    cw = [_ext(nc, f"cw{l}", [128, 4, 31], F32) for l in range(DEPTH)]
    cvec = [_ext(nc, f"cvec{l}", [128, 3, 4], F32) for l in range(DEPTH)]
    w_pw = [_ext(nc, f"w_pw{l}", [512, 512], F32) for l in range(DEPTH)]
    w_out = [_ext(nc, f"w_out{l}", [D, D], F32) for l in range(DEPTH)]
    wgt = _ext(nc, "w_gate", [D, D_FF], F32)
    wup = _ext(nc, "w_up", [D, D_FF], F32)
    wdn = _ext(nc, "w_down", [D_FF, D], F32)
    wr = _ext(nc, "w_router", [D, NE], F32)
    eg = _ext(nc, "e_gate", [NE, D, FE], F32)
    eu = _ext(nc, "e_up", [NE, D, FE], F32)
    ed = _ext(nc, "e_down", [NE, FE, D], F32)
    hT_own_t = nc.dram_tensor("hT_own", [D, TOK], BF16)
    hT_g_t = nc.dram_tensor("hT_g", [2 * D, TOK], BF16)
    oT_h_t = nc.dram_tensor("oT_h", [768, SEQ], BF16)
    oT_g_t = nc.dram_tensor("oT_g", [1536, SEQ], BF16)
    hT_own, hT_g, oT_h, oT_g = hT_own_t.ap(), hT_g_t.ap(), oT_h_t.ap(), oT_g_t.ap()
    cvT = _int(nc, "cvT", [512, TOK], BF16)
    x1 = _int(nc, "x1", [TOK, D], F32)
    h2T = _int(nc, "h2T", [D, TOK], BF16)
    comb = _int(nc, "comb", [TOK, NE], F32)
    aT_scr = _int(nc, "aT_scr", [16, 128, NFC, 128], BF16)
    xA = _int(nc, "xA", [TOK, D], F32)
    xB = _int(nc, "xB", [TOK, D], F32)

    S = Sched(nc)
    C = declare_consts(nc, S, H_CONSTS)
    C["eps"] = S.sbuf("epsc", [128, 1], F32)
    S.op("pool", lambda e: e.memset(C["eps"][:], EPS), writes=[C["eps"]])
    S.barrier()
    with S.scope():
        phase_norm(S, C, x_in, g_attn[0], hT_own)
    S.barrier()
    ncc = [0]

    def allgather(src_t, dst_t, npieces, rows):
        S.barrier()
        for k in range(npieces):
            S.op("pool", lambda e, k=k: e.collective_compute(
                "AllGather", ALU.bypass, replica_groups=RG_PAIR,
                ins=[src_t.ap()[k * rows:(k + 1) * rows, :].opt()], outs=[dst_t.ap()[2 * k * rows:2 * (k + 1) * rows, :].opt()]),
                dma="cc", inc=1)
        S.barrier()

    def hsrc(tg):
        rank, loc = tg // 8, tg % 8
        return [(4 * k, 4, hT_g[k * 1024 + rank * 512:k * 1024 + (rank + 1) * 512, loc * 256:(loc + 1) * 256].rearrange("(c p) t -> p c t", p=128))
                for k in range(4)]

    halo = [(4 * k, 4, hT_g[k * 1024:k * 1024 + 512, TOK - 32:TOK]) for k in range(4)]

    oT_mine = _int(nc, "oT_mine", [1536, TOK], BF16)
    rowchunk = lambda rank, fc: (fc // 2) * 4 + rank * 2 + (fc % 2)
    OMAP = [(h, 1, rowchunk(h // 3, h % 3)) for h in range(6)] + [(10 + h, 1, rowchunk(h // 3, 3 + h % 3)) for h in range(6)]

    def copy_mine():
        def f(e):
            pid = e.partition_id()
            r = pid % 2
            return e.dma_start(out=oT_mine, in_=oT_g[:, bass.ds(r * TOK, TOK)])
        S.op("pool", f, dma="cp_o")
        S.barrier()

    x_cur = x_in
    for l in range(DEPTH):
        last = (l == DEPTH - 1)
        moe = (l % 2 == 1)
        allgather(hT_own_t, hT_g_t, 4, 512)
        with S.scope():
            phase_heads(S, C, hsrc, w_sb[l], w_da[l], lam[l], dg[l], oT_h, l)
        allgather(oT_h_t, oT_g_t, 3, 256)
        copy_mine()
        phase_conv(S, C, hT_own, halo, flag, w_glu[l], cw[l], cvec[l], w_pw[l], cvT)
        S.barrier()
        phase_outproj(S, C, x_cur, oT_mine, cvT, w_out[l], g_ffn[l], x1, h2T,
                      wr_dram=(wr if moe else None), comb_dram=(comb if moe else None), omap=OMAP)
        S.barrier()
        x2 = xB if moe else xA
        if moe:
            experts = [(eg[e], eu[e], ed[e], e) for e in range(NE)]
        else:
            experts = [(wgt[:, h * FE:(h + 1) * FE], wup[:, h * FE:(h + 1) * FE], wdn[h * FE:(h + 1) * FE, :], None) for h in range(2)]
        phase_ffn(S, C, h2T, x1, x2, experts, comb, aT_scr)
        S.barrier()
        if last:
            phase_final_norm(S, C, x2, g_fin, out)
        else:
            with S.scope():
                phase_norm(S, C, x2, g_attn[l + 1], hT_own)
        S.barrier()
        x_cur = x2
    S.emit()
    return nc


def fused_inputs(c, x, attn_norm, w_in, w_out, lam, diff_norm, conv_w, conv_b, conv_ln_g, conv_ln_b,
                 w_conv_out, ffn_norm, w_gate, w_up, w_down, w_router, e_gate, e_up, e_down, final_norm, shared):
    A = lambda a: np.ascontiguousarray(np.asarray(a, dtype=np.float32))
    b, r = c // 2, c % 2
    hcst = host_consts(r)
    m = {"x": A(x[b, r * TOK:(r + 1) * TOK]), "flag": np.full((128, 1), float(r), np.float32)}
    for k in H_CONSTS:
        m[k] = hcst[k]
    hs = [3 * r + i for i in range(3)]
    cols = lambda base: np.concatenate([np.arange(base + h * 128, base + (h + 1) * 128) for h in hs])
    for l in range(DEPTH):
        wl = np.asarray(w_in[l])
        m[f"w_sb{l}"] = A(np.concatenate([wl[:, cols(0)], wl[:, cols(768)], wl[:, cols(1536)]], axis=1))
        m[f"w_da{l}"] = A(np.concatenate([wl[:, cols(3328)], wl[:, cols(4096)], wl[:, cols(4864)]], axis=1))
        m[f"diffg{l}"] = A(np.asarray(diff_norm[l]).reshape(6, 128)[3 * r:3 * r + 3].T)
    m.update(shared)
    return m


def shared_inputs(attn_norm, w_in, w_out, lam, conv_w, conv_b, conv_ln_g, conv_ln_b,
                  w_conv_out, ffn_norm, w_gate, w_up, w_down, w_router, e_gate, e_up, e_down, final_norm):
    A = lambda a: np.ascontiguousarray(np.asarray(a, dtype=np.float32))
    m = {"g_final": A(final_norm), "w_gate": A(w_gate[0]), "w_up": A(w_up[0]), "w_down": A(w_down[0]),
         "w_router": A(w_router[0]), "e_gate": A(e_gate[0]), "e_up": A(e_up[0]), "e_down": A(e_down[0])}
    for l in range(DEPTH):
        wl = np.asarray(w_in[l])
        m[f"g_attn{l}"] = A(attn_norm[l]); m[f"g_ffn{l}"] = A(ffn_norm[l]); m[f"lam{l}"] = A(lam[l])
        m[f"w_glu{l}"] = A(wl[:, 2304:3328])
        m[f"cw{l}"] = A(np.asarray(conv_w[l]).T.reshape(4, 128, 31).transpose(1, 0, 2))
        m[f"cvec{l}"] = A(np.stack([np.asarray(conv_b[l]).reshape(4, 128).T, np.asarray(conv_ln_g[l]).reshape(4, 128).T,
                                    np.asarray(conv_ln_b[l]).reshape(4, 128).T], axis=1))
        m[f"w_pw{l}"] = A(w_conv_out[l]); m[f"w_out{l}"] = A(w_out[l])
    return m


_NC_CACHE = {}


def _get(name, fn):
    if name not in _NC_CACHE:
        _NC_CACHE[name] = fn()
    return _NC_CACHE[name]


def kernel(x, attn_norm, w_in, w_out, lam, diff_norm, conv_w, conv_b, conv_ln_g, conv_ln_b,
           w_conv_out, ffn_norm, w_gate, w_up, w_down, w_router, e_gate, e_up, e_down, final_norm):
    x = np.asarray(x, dtype=np.float32)
    shared = shared_inputs(attn_norm, w_in, w_out, lam, conv_w, conv_b, conv_ln_g, conv_ln_b,
                           w_conv_out, ffn_norm, w_gate, w_up, w_down, w_router, e_gate, e_up, e_down, final_norm)
    ins = [fused_inputs(c, x, attn_norm, w_in, w_out, lam, diff_norm, conv_w, conv_b, conv_ln_g, conv_ln_b,
                        w_conv_out, ffn_norm, w_gate, w_up, w_down, w_router, e_gate, e_up, e_down, final_norm, shared)
           for c in range(8)]
    nc = _get("fused", build_fused)
    res = run_bass_kernel_spmd(nc, ins, core_ids=list(range(8))).results
    out_full = np.zeros((NB, SEQ, D), np.float32)
    for c in range(8):
        out_full[c // 2, (c % 2) * TOK:(c % 2 + 1) * TOK] = res[c]["out"]
    return out_full
```

```python
import contextlib
import numpy as np
import concourse.bass as bass
import concourse.mybir as mybir

F32 = mybir.dt.float32
BF16 = mybir.dt.bfloat16
AF = mybir.ActivationFunctionType
ALU = mybir.AluOpType
AX = mybir.AxisListType

ENGS = ["pe", "act", "dve", "pool", "sp"]


class Buf:
    def __init__(self, name, t=None):
        self.name = name
        self.t = t
        self.w = None
        self.readers = []

    def __getitem__(self, k):
        return self.t[k]


class Sched:
    def __init__(self, nc, same_engine_sync=True):
        self.nc = nc
        self.prog = {e: [] for e in ENGS}
        self.cnt = {}
        self.waited = {}
        self.same = same_engine_sync
        self.stack = contextlib.ExitStack()
        self.stacks = [self.stack]
        self.ntile = 0

    def sbuf(self, name, shape, dtype):
        self.ntile += 1
        t = self.stacks[-1].enter_context(self.nc.sbuf_tensor(f"{name}_{self.ntile}", list(shape), dtype))
        return Buf(name, t)

    @contextlib.contextmanager
    def scope(self):
        st = contextlib.ExitStack()
        self.stacks.append(st)
        try:
            yield
        finally:
            self.barrier()
            self.stacks.pop()
            st.close()

    def psum(self, name, shape, dtype=F32):
        self.ntile += 1
        t = self.stacks[-1].enter_context(self.nc.psum_tensor(f"{name}_{self.ntile}", list(shape), dtype))
        return Buf(name, t)

    def _deps(self, reads, writes):
        deps = {}
        def add(d):
            if d is None:
                return
            k, v = d
            if k.startswith("ld_c"):
                v = self.cnt[k]
            if deps.get(k, 0) < v:
                deps[k] = v
        for b in reads:
            add(b.w)
        for b in writes:
            add(b.w)
            for r in b.readers:
                add(r)
        return deps

    def op(self, eng, fn, reads=(), writes=(), dma=None, n=1, inc=None):
        deps = self._deps(reads, writes)
        for k, v in deps.items():
            if dma is None and k == eng:
                if eng == "pe" or not self.same:
                    continue
            if self.waited.get((eng, k), 0) >= v:
                continue
            self.waited[(eng, k)] = v
            self.prog[eng].append(("wait", k, v))
        key = eng if dma is None else dma
        inc = inc if inc is not None else (1 if dma is None else 16)
        self.cnt[key] = self.cnt.get(key, 0) + inc * n
        val = self.cnt[key]
        self.prog[eng].append(("op", fn, key, inc, n))
        for b in reads:
            b.readers.append((key, val))
        for b in writes:
            b.w = (key, val)
            b.readers = []
        return val

    def wait_all(self, eng, keys=None):
        for k, v in self.cnt.items():
            if keys is not None and k not in keys:
                continue
            if k == eng:
                continue
            if self.waited.get((eng, k), 0) >= v:
                continue
            self.waited[(eng, k)] = v
            self.prog[eng].append(("wait", k, v))

    def barrier(self):
        for e in ENGS:
            self.wait_all(e)

    def emit(self):
        nc = self.nc
        sems = {}
        for k in self.cnt:
            sems[k] = self.stack.enter_context(nc.semaphore(f"s_{k}"))
        prog = self.prog

        def run(e, lst):
            for item in lst:
                if item[0] == "wait":
                    e.wait_ge(sems[item[1]], item[2])
                else:
                    _, fn, key, inc, _n = item
                    r = fn(e)
                    if isinstance(r, (list, tuple)):
                        for ins in r:
                            ins.then_inc(sems[key], inc)
                    else:
                        r.then_inc(sems[key], inc)

        with nc.Block() as block:
            block.tensor(lambda e: run(e, prog["pe"]))
            block.scalar(lambda e: run(e, prog["act"]))
            block.vector(lambda e: run(e, prog["dve"]))
            block.gpsimd(lambda e: run(e, prog["pool"]))
            block.sync(lambda e: run(e, prog["sp"]))
        self.stack.close()


D = 2048
SEQ = 4096
NB = 4
TOK = 2048
EPS = 1e-6
IN_W = 5632
D_FF = 5632
D_FFE = 2816
NE = 8


def ring(lst, i):
    return lst[i % len(lst)]


def emit_norm_tile(S, C, xt, gbc, hstage, col0, ps_ring, it, extra=None):
    junk = ring(C["junk"], it)
    ss = ring(C["ss"], it)
    rstd = ring(C["rstd"], it)
    hb = ring(C["hb"], it)
    S.op("act", lambda e: e.activation(out=junk[:], in_=xt[:], func=AF.Square, accum_out=ss[:]),
         reads=[xt], writes=[junk, ss])
    S.op("act", lambda e: e.activation(out=rstd[:], in_=ss[:], func=AF.Sqrt, scale=1.0 / D, bias=C["eps"][:, 0:1]),
         reads=[ss, C["eps"]], writes=[rstd])
    S.op("dve", lambda e: e.reciprocal(out=rstd[:], in_=rstd[:]), reads=[rstd], writes=[rstd])
    if extra is not None:
        hf = extra
        S.op("dve", lambda e: e.scalar_tensor_tensor(out=hf[:], in0=xt[:], scalar=rstd[:, 0:1], in1=gbc[:],
                                                     op0=ALU.mult, op1=ALU.mult),
             reads=[xt, rstd, gbc], writes=[hf])
        S.op("pool", lambda e: e.tensor_copy(out=hb[:], in_=hf[:]), reads=[hf], writes=[hb])
    else:
        S.op("dve", lambda e: e.scalar_tensor_tensor(out=hb[:], in0=xt[:], scalar=rstd[:, 0:1], in1=gbc[:],
                                                     op0=ALU.mult, op1=ALU.mult),
             reads=[xt, rstd, gbc], writes=[hb])
    ident = C["ident_bf"]
    for half in range(2):
        ps = ring(ps_ring, it * 2 + half)
        for k in range(8):
            c = half * 8 + k
            S.op("pe", lambda e, c=c, k=k, ps=ps: e.transpose(out=ps[:, k * 128:(k + 1) * 128],
                                                               in_=hb[:, c * 128:(c + 1) * 128],
                                                               identity=ident[:]),
                 reads=[hb, ident], writes=[ps])
        eng = "act" if half == 0 else "dve"
        if eng == "act":
            S.op("act", lambda e, ps=ps, half=half: e.activation(
                out=hstage[:, half * 8:(half + 1) * 8, col0:col0 + 128],
                in_=ps[:].rearrange("p (c t) -> p c t", c=8), func=AF.Copy),
                reads=[ps], writes=[hstage])
        else:
            S.op("dve", lambda e, ps=ps, half=half: e.tensor_copy(
                out=hstage[:, half * 8:(half + 1) * 8, col0:col0 + 128],
                in_=ps[:].rearrange("p (c t) -> p c t", c=8)),
                reads=[ps], writes=[hstage])
    return rstd


def norm_ctx(S, consts):
    C = dict(consts)
    C["eps"] = S.sbuf("epsc", [128, 1], F32)
    S.op("pool", lambda e: e.memset(C["eps"][:], EPS), writes=[C["eps"]])
    C["junk"] = [S.sbuf("junk", [128, D], BF16) for _ in range(1)]
    C["ss"] = [S.sbuf("ss", [128, 1], F32) for _ in range(2)]
    C["rstd"] = [S.sbuf("rstd", [128, 1], F32) for _ in range(2)]
    C["hb"] = [S.sbuf("hb", [128, D], BF16) for _ in range(2)]
    return C


def phase_norm(S, consts, x_dram, g_dram, hT_dram):
    C = norm_ctx(S, consts)
    gbc = S.sbuf("gbc", [128, D], F32)
    S.op("sp", lambda e: e.dma_start(out=gbc[:], in_=g_dram.partition_broadcast(128)), writes=[gbc], dma="ld_c_g")
    xs = [S.sbuf("xs", [128, D], F32) for _ in range(2)]
    hst = [S.sbuf("hst", [128, 16, 512], BF16) for _ in range(2)]
    ps_ring = [S.psum("pst", [128, 1024], BF16) for _ in range(2)]
    hT_v = hT_dram.rearrange("(c p) t -> p c t", p=128)
    def ldx(tt):
        xt = ring(xs, tt)
        S.op("sp", lambda e, xt=xt, tt=tt: e.dma_start(out=xt[:], in_=x_dram[tt * 128:(tt + 1) * 128, :]),
             writes=[xt], dma=f"ld_x{tt % 2}")
    ldx(0)
    for tt in range(TOK // 128):
        xt = ring(xs, tt)
        if tt + 1 < TOK // 128:
            ldx(tt + 1)
        hstage = ring(hst, tt // 4)
        emit_norm_tile(S, C, xt, gbc, hstage, (tt % 4) * 128, ps_ring, tt)
        if tt % 4 == 3:
            g = tt // 4
            S.op("sp", lambda e, hstage=hstage, g=g: e.dma_start(out=hT_v[:, :, g * 512:(g + 1) * 512], in_=hstage[:]),
                 reads=[hstage], dma=f"st_h{g % 2}")


def load_cast(S, C, dst, dst_ap, src_ap, shape, it, eng="pool"):
    st = ring(C["wstage"], it)
    a, b = shape[1], shape[2]
    view = lambda: st[:, 0:a * b].rearrange("p (a b) -> p a b", a=a)
    S.op("sp", lambda e: e.dma_start(out=view(), in_=src_ap), writes=[st], dma=f"ld_w{it % len(C['wstage'])}")
    if eng == "act":
        S.op("act", lambda e: e.activation(out=dst_ap, in_=view(), func=AF.Copy), reads=[st], writes=[dst])
    else:
        S.op(eng, lambda e: e.tensor_copy(out=dst_ap, in_=view()), reads=[st], writes=[dst])


def evac(S, it, out_ap, out_buf, ps, ps_ap, scale=None):
    if it % 2 == 0:
        if scale is None:
            S.op("act", lambda e: e.activation(out=out_ap, in_=ps_ap, func=AF.Copy), reads=[ps], writes=[out_buf])
        else:
            S.op("act", lambda e: e.activation(out=out_ap, in_=ps_ap, func=AF.Copy, scale=scale), reads=[ps], writes=[out_buf])
    else:
        if scale is None:
            S.op("dve", lambda e: e.tensor_copy(out=out_ap, in_=ps_ap), reads=[ps], writes=[out_buf])
        else:
            S.op("dve", lambda e: e.tensor_scalar(out=out_ap, in0=ps_ap, scalar1=scale, scalar2=None, op0=ALU.mult),
                 reads=[ps], writes=[out_buf])


SB_SCALE = 128 ** -0.5
DEBUG_SKIP = set()
NQG = SEQ // 256


def proj_heads(S, C, hsrc, w_dram, pb, kscale, split_q=False):
    if split_q:
        qT = [S.sbuf("qT", [128, 2, SEQ], BF16) for _ in range(3)]
        for hl in range(3):
            S.op("pool", lambda e, hl=hl: e.memset(qT[hl][:], 0.0), writes=[qT[hl]])
    else:
        qT = [S.sbuf("qT", [128, SEQ], BF16) for _ in range(3)]
    kT = [S.sbuf("kT", [128, SEQ], BF16) for _ in range(3)]
    v = S.sbuf("v", [128, SEQ // 128, 384], BF16)
    with S.scope():
        w = S.sbuf("wproj", [128, 16, 1152], BF16)
        C2 = dict(C)
        C2["wstage"] = [S.sbuf("wstage", [128, 2048], F32) for _ in range(2)]
        for i in range(9):
            load_cast(S, C2, w, w[:, :, i * 128:(i + 1) * 128],
                      w_dram[:, i * 128:(i + 1) * 128].rearrange("(c p) n -> p c n", p=128), [128, 16, 128], i,
                      eng=["dve", "act", "pool"][i % 3])
        hts = [S.sbuf("hts", [128, 16, 256], BF16) for _ in range(2)]
        ei = 0
        pi = 0
        def ldh(tg):
            ht = ring(hts, tg)
            pieces = hsrc(tg)
            S.op("sp", lambda e, ht=ht, pieces=pieces: [e.dma_start(out=ht[:, c0:c0 + n_, :], in_=ap_) for (c0, n_, ap_) in pieces],
                 writes=[ht], dma=f"ld_h{tg % 2}", n=len(pieces))
        ldh(0)
        for tg in range(SEQ // 256):
            ht = ring(hts, tg)
            if tg + 1 < SEQ // 256:
                ldh(tg + 1)
            for which in range(2):
                for hl in range(3):
                    ps = ring(pb, pi); pi += 1
                    col = which * 384 + hl * 128
                    for c in range(16):
                        S.op("pe", lambda e, ps=ps, c=c, col=col, ht=ht: e.matmul(
                            ps[:, 0:256], lhsT=w[:, c, col:col + 128], rhs=ht[:, c, :], start=(c == 0), stop=(c == 15)),
                            reads=[w, ht], writes=[ps])
                    dst = (qT if which == 0 else kT)[hl]
                    if which == 0 and split_q:
                        evac(S, 0, dst[0:64, 0, tg * 256:(tg + 1) * 256], dst, ps, ps[0:64, 0:256])
                        evac(S, 1, dst[64:128, 1, tg * 256:(tg + 1) * 256], dst, ps, ps[64:128, 0:256])
                    else:
                        evac(S, ei, dst[:, tg * 256:(tg + 1) * 256], dst, ps, ps[:, 0:256],
                             scale=(kscale if (which == 1 and kscale is not None) else None))
                    ei += 1
            for tt in range(2):
                ps = ring(pb, pi); pi += 1
                for c in range(16):
                    S.op("pe", lambda e, ps=ps, c=c, tt=tt, ht=ht: e.matmul(
                        ps[:, 0:384], lhsT=ht[:, c, tt * 128:(tt + 1) * 128], rhs=w[:, c, 768:1152],
                        start=(c == 0), stop=(c == 15)), reads=[w, ht], writes=[ps])
                evac(S, ei, v[:, tg * 2 + tt, :], v, ps, ps[:, 0:384])
                ei += 1
    return qT, kT, v


def attn_sb(S, C, qT, kT, v, oT, pb):
    Aring = [pb[0], pb[1]]
    Bb = [pb[2], pb[3], pb[4]]
    Cc = [pb[5], pb[6], pb[7]]
    e_sb = [[S.sbuf("e", [128, 512], F32) for _ in range(2)] for _ in range(3)]
    sp_sb = [[S.sbuf("sp", [128, 512], BF16) for _ in range(3)] for _ in range(3)]
    spm_sb = [[S.sbuf("spm", [128, 512], BF16) for _ in range(3)] for _ in range(3)]
    w_sb = [[S.sbuf("w", [128, 512], BF16) for _ in range(3)] for _ in range(3)]
    wm_sb = [[S.sbuf("wm", [128, 512], BF16) for _ in range(3)] for _ in range(3)]
    R = [[S.sbuf("R", [128, 512], BF16) for _ in range(2)] for _ in range(3)]
    negtri, negones, mask = C["negtri"], C["negones"], C["sbmask"]
    tiles = []
    for gq in range(SEQ // 512):
        jlist = list(range(4 * gq + 3, -1, -1))
        for n, j in enumerate(jlist):
            tiles.append(dict(gq=gq, j=j, n=n, first=(n == 0), last=(n == len(jlist) - 1), it=len(tiles)))
    st = {}

    def stageA(t):
        it, j, gq = t["it"], t["j"], t["gq"]
        qs = slice(gq * 512, (gq + 1) * 512); ks = slice(j * 128, (j + 1) * 128)
        diag = j >= 4 * gq
        sps = {}
        for hl in range(3):
            A = ring(Aring, it * 3 + hl)
            S.op("pe", lambda e, A=A, hl=hl, ks=ks, qs=qs: e.matmul(A[:, 0:512], lhsT=kT[hl][:, ks], rhs=qT[hl][:, qs], start=True, stop=True),
                 reads=[kT[hl], qT[hl]], writes=[A])
            eb = ring(e_sb[hl], it)
            S.op("act", lambda e, A=A, eb=eb: e.activation(out=eb[:], in_=A[:, 0:512], func=AF.Exp), reads=[A], writes=[eb])
            spb = ring(sp_sb[hl], it)
            S.op("act", lambda e, eb=eb, spb=spb: e.activation(out=spb[:], in_=eb[:], func=AF.Ln, bias=C["one"][:, 0:1]),
                 reads=[eb, C["one"]], writes=[spb])
            if diag:
                spm = ring(spm_sb[hl], it)
                jj = j - 4 * gq
                S.op("pool", lambda e, spm=spm, spb=spb, jj=jj: e.tensor_tensor(out=spm[:], in0=spb[:], in1=mask[:, jj, :], op=ALU.mult),
                     reads=[spb, mask], writes=[spm])
                spb = spm
            sps[hl] = spb
        st[it] = {"sp": sps}

    def stageB(t):
        it, j, gq, n, first, last = t["it"], t["j"], t["gq"], t["n"], t["first"], t["last"]
        qs = slice(gq * 512, (gq + 1) * 512); ks = slice(j * 128, (j + 1) * 128)
        diag = j >= 4 * gq
        ws = {}
        for hl in range(3):
            B = Bb[hl]
            spb = st[it]["sp"][hl]
            Rprev = ring(R[hl], n - 1)
            Rnew = ring(R[hl], n)
            S.op("pe", lambda e, B=B, hl=hl, ks=ks, qs=qs: e.matmul(B[:, 0:512], lhsT=kT[hl][:, ks], rhs=qT[hl][:, qs], start=True, stop=False),
                 reads=[kT[hl], qT[hl]], writes=[B])
            S.op("pe", lambda e, B=B, spb=spb, first=first: e.matmul(B[:, 0:512], lhsT=negtri[:], rhs=spb[:], start=False, stop=first),
                 reads=[negtri, spb], writes=[B])
            if not first:
                S.op("pe", lambda e, B=B, Rprev=Rprev: e.matmul(B[:, 0:512], lhsT=negones[:], rhs=Rprev[:], start=False, stop=True),
                     reads=[negones, Rprev], writes=[B])
            if not last:
                if first:
                    S.op("dve", lambda e, Rnew=Rnew, spb=spb: e.tensor_copy(out=Rnew[:], in_=spb[:]), reads=[spb], writes=[Rnew])
                else:
                    S.op("dve", lambda e, Rnew=Rnew, Rprev=Rprev, spb=spb: e.tensor_tensor(out=Rnew[:], in0=Rprev[:], in1=spb[:], op=ALU.add),
                         reads=[spb, Rprev], writes=[Rnew])
            wb = ring(w_sb[hl], it)
            S.op("act", lambda e, B=B, wb=wb: e.activation(out=wb[:], in_=B[:, 0:512], func=AF.Exp), reads=[B], writes=[wb])
            if diag:
                wm = ring(wm_sb[hl], it)
                jj = j - 4 * gq
                S.op("pool", lambda e, wm=wm, wb=wb, jj=jj: e.tensor_tensor(out=wm[:], in0=wb[:], in1=mask[:, jj, :], op=ALU.mult),
                     reads=[wb, mask], writes=[wm])
                wb = wm
            ws[hl] = wb
        st[it]["w"] = ws

    def stageC(t):
        it, j, gq, first, last = t["it"], t["j"], t["gq"], t["first"], t["last"]
        qs = slice(gq * 512, (gq + 1) * 512)
        for hl in range(3):
            wb = st[it]["w"][hl]
            S.op("pe", lambda e, hl=hl, wb=wb, j=j, first=first, last=last: e.matmul(
                Cc[hl][:, 0:512], lhsT=v[:, j, hl * 128:(hl + 1) * 128], rhs=wb[:], start=first, stop=last),
                reads=[v, wb], writes=[Cc[hl]])
        if last:
            for hl in range(3):
                evac(S, gq * 3 + hl, oT[:, hl, qs], oT, Cc[hl], Cc[hl][:, 0:512])
        del st[it]

    stages = [stageA, stageB, stageC]
    for step in range(len(tiles) + len(stages) - 1):
        for si, stg in enumerate(stages):
            idx = step - si
            if 0 <= idx < len(tiles):
                stg(tiles[idx])


def attn_da(S, C, qT, kT, v, oT, pb, lam_init):
    Pr = [pb[0], pb[1]]
    O = [pb[2], pb[3], pb[4]]
    N = [pb[5], pb[6], pb[7]]
    E_sb = [[S.sbuf("E", [128, 512], BF16) for _ in range(3)] for _ in range(3)]
    Ef_sb = [[S.sbuf("Ef", [128, 512], F32) for _ in range(2)] for _ in range(3)]
    recs = [S.sbuf("rec", [128, 512], F32) for _ in range(3)]
    o12s = [S.sbuf("o12", [128, 512], F32) for _ in range(3)]
    obs = [S.sbuf("ob", [128, 256], F32) for _ in range(3)]
    osqs = [S.sbuf("osq", [128, 256], F32) for _ in range(3)]
    rss = [S.sbuf("rs", [128, 256], F32) for _ in range(3)]
    pending = []
    bias, maskF, ones_bf, onesf = C["dabias"], C["damask"], C["ones_bf"], C["onesf128"]
    neglam, gsc = C["neglam"], C["gscale"]
    tiles = []
    for gq in range(NQG):
        jlist = list(range(2 * gq + 1, -1, -1))
        for n, j in enumerate(jlist):
            tiles.append(dict(gq=gq, j=j, n=n, first=(n == 0), last=(n == len(jlist) - 1), it=len(tiles)))
    st = {}

    def stageA(t, hl):
        it, j, gq = t["it"], t["j"], t["gq"]
        qs = slice(gq * 256, (gq + 1) * 256); ks = slice(j * 128, (j + 1) * 128)
        diag = j >= 2 * gq
        cidx = j - 2 * gq - 1 + 32
        Es = st.setdefault(it, {})
        if True:
            P = ring(Pr, it * 3 + hl)
            for m in range(2):
                S.op("pe", lambda e, P=P, hl=hl, m=m, ks=ks, qs=qs: e.matmul(
                    P[:, m * 256:(m + 1) * 256], lhsT=kT[hl][:, ks], rhs=qT[hl][:, m, qs],
                    start=True, stop=True), reads=[kT[hl], qT[hl]], writes=[P])
            Eb = ring(E_sb[hl], it)
            if diag:
                Ef = ring(Ef_sb[hl], it)
                jj = j - 2 * gq
                S.op("act", lambda e, P=P, Ef=Ef, hl=hl, cidx=cidx: e.activation(
                    out=Ef[:], in_=P[:], func=AF.Exp, scale=0.125, bias=bias[:, hl, cidx:cidx + 1]),
                    reads=[P, bias], writes=[Ef])
                S.op("dve", lambda e, Ef=Ef, Eb=Eb, hl=hl, jj=jj: e.tensor_tensor(
                    out=Eb[:].rearrange("p (m t) -> p m t", m=2), in0=Ef[:].rearrange("p (m t) -> p m t", m=2),
                    in1=maskF[:, hl, jj:jj + 1, :].to_broadcast([128, 2, 256]), op=ALU.mult),
                    reads=[Ef, maskF], writes=[Eb])
            else:
                S.op("act", lambda e, P=P, Eb=Eb, hl=hl, cidx=cidx: e.activation(
                    out=Eb[:], in_=P[:], func=AF.Exp, scale=0.125, bias=bias[:, hl, cidx:cidx + 1]),
                    reads=[P, bias], writes=[Eb])
            Es[hl] = Eb

    def stageB(t, hl):
        it, j, gq, n, first, last = t["it"], t["j"], t["gq"], t["n"], t["first"], t["last"]
        qs = slice(gq * 256, (gq + 1) * 256)
        if True:
            Eb = st[it][hl]
            S.op("pe", lambda e, hl=hl, Eb=Eb, j=j, first=first, last=last: e.matmul(
                O[hl][:], lhsT=v[:, j, hl * 128:(hl + 1) * 128], rhs=Eb[:], start=first, stop=last),
                reads=[v, Eb], writes=[O[hl]])
            S.op("pe", lambda e, hl=hl, Eb=Eb, first=first, last=last: e.matmul(
                N[hl][:], lhsT=ones_bf[:], rhs=Eb[:], start=first, stop=last),
                reads=[ones_bf, Eb], writes=[N[hl]])
        if hl < 2:
            return
        del st[it]
        if n == 2 and pending:
            pending.pop()()
        if not last:
            return
        for hl in range(3):
            S.op("dve", lambda e, hl=hl: e.reciprocal(out=recs[hl][:], in_=N[hl][:]), reads=[N[hl]], writes=[recs[hl]])
            S.op("dve", lambda e, hl=hl: e.tensor_tensor(out=o12s[hl][:], in0=O[hl][:], in1=recs[hl][:], op=ALU.mult),
                 reads=[O[hl], recs[hl]], writes=[o12s[hl]])

        def rest(qs=qs, gq=gq):
            for hl in range(3):
                o12, ob, osq, rs = o12s[hl], obs[hl], osqs[hl], rss[hl]
                S.op("dve", lambda e, o12=o12, ob=ob: e.scalar_tensor_tensor(out=ob[:], in0=o12[:, 256:512], scalar=neglam[:, 0:1], in1=o12[:, 0:256],
                                                                             op0=ALU.mult, op1=ALU.add), reads=[o12, neglam], writes=[ob])
                S.op("pool", lambda e, ob=ob, osq=osq: e.tensor_tensor(out=osq[:], in0=ob[:], in1=ob[:], op=ALU.mult), reads=[ob], writes=[osq])
                P = ring(Pr, gq * 3 + hl)
                S.op("pe", lambda e, P=P, osq=osq: e.matmul(P[:, 0:256], lhsT=onesf[:], rhs=osq[:], start=True, stop=True),
                     reads=[onesf, osq], writes=[P])
                S.op("act", lambda e, P=P, rs=rs: e.activation(out=rs[:], in_=P[:, 0:256], func=AF.Sqrt, bias=C["eps"][:, 0:1]),
                     reads=[P, C["eps"]], writes=[rs])
                S.op("dve", lambda e, rs=rs: e.reciprocal(out=rs[:], in_=rs[:]), reads=[rs], writes=[rs])
                S.op("dve", lambda e, hl=hl, qs=qs, ob=ob, rs=rs: e.scalar_tensor_tensor(out=oT[:, hl, qs], in0=ob[:], scalar=gsc[:, hl:hl + 1], in1=rs[:],
                                                                                     op0=ALU.mult, op1=ALU.mult), reads=[ob, gsc, rs], writes=[oT])
        while pending:
            pending.pop()()
        pending.append(rest)

    for step in range(len(tiles) + 1):
        for hl in range(3):
            if step < len(tiles):
                stageA(tiles[step], hl)
            if step >= 1:
                stageB(tiles[step - 1], hl)
    while pending:
        pending.pop()()


def da_scalars(S, C, lam_dram, diffg_dram, pb, lam_init):
    lamt = S.sbuf("lamt", [1, 256], F32)
    S.op("sp", lambda e: e.dma_start(out=lamt[:], in_=lam_dram.rearrange("(o a) b -> o (a b)", o=1)), writes=[lamt], dma="ld_c_lam")
    prod = S.sbuf("lprod", [1, 128], F32)
    S.op("dve", lambda e: e.tensor_tensor(out=prod[:].rearrange("p (a b) -> p a b", a=2),
                                          in0=lamt[:].rearrange("p (a t b) -> p a t b", a=2, t=2)[:, :, 0, :],
                                          in1=lamt[:].rearrange("p (a t b) -> p a t b", a=2, t=2)[:, :, 1, :], op=ALU.mult),
         reads=[lamt], writes=[prod])
    sums = S.sbuf("lsum", [1, 2], F32)
    S.op("dve", lambda e: e.reduce_sum(out=sums[:], in_=prod[:].rearrange("p (a b) -> p a b", a=2), axis=AX.X),
         reads=[prod], writes=[sums])
    ex = S.sbuf("lex", [1, 2], F32)
    S.op("act", lambda e: e.activation(out=ex[:], in_=sums[:], func=AF.Exp), reads=[sums], writes=[ex])
    nl = S.sbuf("nl", [1, 1], F32)
    S.op("dve", lambda e: e.tensor_tensor(out=nl[:], in0=ex[:, 1:2], in1=ex[:, 0:1], op=ALU.subtract), reads=[ex], writes=[nl])
    S.op("dve", lambda e: e.tensor_scalar(out=nl[:], in0=nl[:], scalar1=-lam_init, scalar2=None, op0=ALU.add), reads=[nl], writes=[nl])
    onesf = C["onesf128"]
    S.op("pe", lambda e: e.matmul(pb[0][:, 0:1], lhsT=onesf[0:1, :], rhs=nl[:], start=True, stop=True),
         reads=[onesf, nl], writes=[pb[0]])
    neglam = S.sbuf("neglam", [128, 1], F32)
    S.op("act", lambda e: e.activation(out=neglam[:], in_=pb[0][:, 0:1], func=AF.Copy, scale=128.0), reads=[pb[0]], writes=[neglam])
    gsc = S.sbuf("gsc", [128, 3], F32)
    S.op("sp", lambda e: e.dma_start(out=gsc[:], in_=diffg_dram), writes=[gsc], dma="ld_c_gsc")
    S.op("dve", lambda e: e.tensor_scalar(out=gsc[:], in0=gsc[:], scalar1=1.0 - lam_init, scalar2=None, op0=ALU.mult), reads=[gsc], writes=[gsc])
    C["neglam"] = neglam
    C["gscale"] = gsc


def load_const(S, name, dram_ap, shape, dtype):
    b = S.sbuf(name, shape, dtype)
    S.op("sp", lambda e: e.dma_start(out=b[:], in_=dram_ap), writes=[b], dma="ld_c")
    return b


def lam_init_of(layer):
    import math
    return 0.8 - 0.6 * math.exp(-0.3 * layer)


def phase_heads(S, C, hsrc, w_sb, w_da, lam_dram, diffg_dram, oT_dram, layer):
    pb = [S.psum("pb", [128, 512], F32) for _ in range(8)]
    lam_init = lam_init_of(layer)
    C = dict(C)
    C["eps"] = S.sbuf("epsc", [128, 1], F32)
    S.op("pool", lambda e: e.memset(C["eps"][:], EPS), writes=[C["eps"]])
    C["one"] = S.sbuf("onec", [128, 1], F32)
    S.op("pool", lambda e: e.memset(C["one"][:], 1.0), writes=[C["one"]])
    da_scalars(S, C, lam_dram, diffg_dram, pb, lam_init)
    S.barrier()
    oview = oT_dram.rearrange("(h p) t -> p h t", p=128)
    for typ in range(2):
        with S.scope():
            oT = S.sbuf("oT", [128, 3, SEQ], BF16)
            qT, kT, v = proj_heads(S, C, hsrc, w_sb if typ == 0 else w_da, pb, SB_SCALE if typ == 0 else None, split_q=(typ == 1))
            S.barrier()
            with S.scope():
                if typ == 0 and "sb" not in DEBUG_SKIP:
                    attn_sb(S, C, qT, kT, v, oT, pb)
                elif typ == 1 and "da" not in DEBUG_SKIP:
                    attn_da(S, C, qT, kT, v, oT, pb, lam_init)
                else:
                    S.op("pool", lambda e, oT=oT: e.memset(oT[:], 0.0), writes=[oT])
            S.op("sp", lambda e, typ=typ, oT=oT: e.dma_start(out=oview[:, typ * 3:(typ + 1) * 3, :], in_=oT[:]), reads=[oT], dma="st_o")
            S.wait_all("sp")


def host_consts(r):
    import ml_dtypes
    bf = ml_dtypes.bfloat16
    s = np.arange(128)[:, None]
    t = np.arange(128)[None, :]
    c = {}
    c["ident_bf"] = np.eye(128, dtype=np.float32).astype(bf)
    c["ident_f"] = np.eye(128, dtype=np.float32)
    c["ones_bf"] = np.ones((128, 128), np.float32).astype(bf)
    c["negones"] = (-np.ones((128, 128), np.float32)).astype(bf)
    c["negtri"] = (-(s >= t).astype(np.float32)).astype(bf)
    c["onesf128"] = np.full((128, 128), 1.0 / 128, np.float32)
    c["onesf512"] = np.full((128, 128), 1.0 / 512, np.float32)
    tl = np.arange(512)[None, :]
    sbm = np.stack([((jj * 128 + s) < tl).astype(np.float32) for jj in range(4)], axis=1)
    c["sbmask"] = sbm.astype(bf)
    heads = np.arange(3) + 3 * r
    slopes = np.exp2(-8.0 * (heads.astype(np.float64) + 1.0) / 6.0)
    cc = np.arange(33) - 32
    c["dabias"] = (slopes[None, :, None] * (np.arange(128)[:, None, None] + 128.0 * cc[None, None, :])).astype(np.float32)
    allowed = ((s // 64) <= (t // 64)).astype(np.float64)
    mf = np.zeros((128, 3, 2, 256), np.float64)
    for hl in range(3):
        F = np.where(s > t, np.exp(-2.0 * slopes[hl] * (s - t)), 1.0) * allowed
        mf[:, hl, 0, 0:128] = F
        mf[:, hl, 0, 128:256] = 1.0
        mf[:, hl, 1, 0:128] = 0.0
        mf[:, hl, 1, 128:256] = F
    c["damask"] = mf.astype(np.float32)
    return c


CONST_SPECS = {
    "ident_bf": ([128, 128], BF16), "ident_f": ([128, 128], F32), "ones_bf": ([128, 128], BF16),
    "negones": ([128, 128], BF16), "negtri": ([128, 128], BF16), "onesf128": ([128, 128], F32),
    "onesf512": ([128, 128], F32), "sbmask": ([128, 4, 512], BF16), "dabias": ([128, 3, 33], F32),
    "damask": ([128, 3, 2, 256], F32),
}


def declare_consts(nc, S, names):
    C = {}
    for nme in names:
        shape, dt_ = CONST_SPECS[nme]
        ap = nc.dram_tensor(nme, shape, dt_, kind="ExternalInput").ap()
        C[nme] = load_const(S, nme, ap, shape, dt_)
    return C


def simulate(S):
    pc = {e: 0 for e in ENGS}
    sem = {}
    total = sum(len(S.prog[e]) for e in ENGS)
    done = 0
    while done < total:
        progressed = False
        for e in ENGS:
            lst = S.prog[e]
            while pc[e] < len(lst):
                it = lst[pc[e]]
                if it[0] == "wait":
                    if sem.get(it[1], 0) >= it[2]:
                        pc[e] += 1; done += 1; progressed = True
                    else:
                        break
                else:
                    sem[it[2]] = sem.get(it[2], 0) + it[3] * it[4] if len(it) > 4 else sem.get(it[2], 0) + it[3]
                    pc[e] += 1; done += 1; progressed = True
        if not progressed:
            info = {e: (pc[e], len(S.prog[e]), S.prog[e][pc[e]][:3] if pc[e] < len(S.prog[e]) else None) for e in ENGS}
            raise RuntimeError(f"DEADLOCK {info} sems={ {k: v for k, v in sem.items()} }")
    for k, v in S.cnt.items():
        assert sem.get(k, 0) == v, (k, sem.get(k, 0), v)
    return {e: len(S.prog[e]) for e in ENGS}


def phase_conv(S, C, hT_own, hT_halo, flag_dram, w_glu, cw_dram, cvec_dram, w_pw, cvT_dram):
    with S.scope():
        pb = [S.psum("pbc", [128, 512], F32) for _ in range(6)]
        C = dict(C)
        wg = S.sbuf("wglu", [128, 16, 1024], BF16)
        wpw = S.sbuf("wpw", [128, 4, 512], BF16)
        with S.scope():
            Cw = {"wstage": [S.sbuf("wstage", [128, 4096], F32) for _ in range(3)]}
            for i in range(4):
                load_cast(S, Cw, wg, wg[:, :, i * 256:(i + 1) * 256],
                          w_glu[:, i * 256:(i + 1) * 256].rearrange("(c p) n -> p c n", p=128), [128, 16, 256], i,
                          eng=["dve", "act", "pool"][i % 3])
            load_cast(S, Cw, wpw, wpw[:], w_pw.rearrange("(c p) n -> p c n", p=128), [128, 4, 512], 4, eng="act")
        cw = S.sbuf("cw", [128, 4, 31], F32)
        cvec = S.sbuf("cvec", [128, 3, 4], F32)
        flag = S.sbuf("flag", [128, 1], F32)
        S.op("sp", lambda e: [e.dma_start(out=cw[:], in_=cw_dram), e.dma_start(out=cvec[:], in_=cvec_dram),
                              e.dma_start(out=flag[:], in_=flag_dram)], writes=[cw, cvec, flag], dma="ld_c_cv", n=3)
        u = S.sbuf("u", [128, 4, 32 + TOK], BF16)
        dgm = S.sbuf("dgm", [128, 4, 31, 128], BF16)
        identb = C["ident_bf"]
        for cc in range(4):
            for k in range(31):
                S.op("dve", lambda e, cc=cc, k=k: e.tensor_scalar(out=dgm[:, cc, k, :], in0=identb[:], scalar1=cw[:, cc, k:k + 1], scalar2=None, op0=ALU.mult), reads=[identb, cw], writes=[dgm])
        y = S.sbuf("y", [128, 4, TOK], F32)
        hv = hT_own.rearrange("(c p) t -> p c t", p=128)
        halo_pieces = hT_halo if isinstance(hT_halo, list) else [(0, 16, hT_halo)]
        halo_pieces = [(c0, n_, ap_.rearrange("(c p) t -> p c t", p=128)) for (c0, n_, ap_) in halo_pieces]
        hts = [S.sbuf("htc", [128, 16, 512], BF16) for _ in range(2)]
        sgs = [S.sbuf("sg", [128, 512], F32) for _ in range(2)]
        pi = 0
        def ldc(grp):
            ht = ring(hts, grp)
            if grp == 0:
                S.op("sp", lambda e, ht=ht: [e.dma_start(out=ht[:, c0:c0 + n_, 0:32], in_=ap_) for (c0, n_, ap_) in halo_pieces],
                     writes=[ht], dma=f"ld_hc{grp % 2}", n=len(halo_pieces))
            else:
                S.op("sp", lambda e, ht=ht, grp=grp: e.dma_start(out=ht[:], in_=hv[:, :, (grp - 1) * 512:grp * 512]),
                     writes=[ht], dma=f"ld_hc{grp % 2}")
        ldc(0)
        for grp in range(5):
            ht = ring(hts, grp)
            if grp + 1 < 5:
                ldc(grp + 1)
            if grp == 0:
                N, off = 32, 0
            else:
                N, off = 512, 32 + (grp - 1) * 512
            for cc in range(4):
                Pa = ring(pb, pi); Pg = ring(pb, pi + 1); pi += 2
                for which, P in ((0, Pa), (1, Pg)):
                    col = which * 512 + cc * 128
                    for c in range(16):
                        S.op("pe", lambda e, P=P, c=c, col=col, ht=ht, N=N: e.matmul(
                            P[:, 0:N], lhsT=wg[:, c, col:col + 128], rhs=ht[:, c, 0:N], start=(c == 0), stop=(c == 15)),
                            reads=[wg, ht], writes=[P])
                sg = ring(sgs, pi // 2)
                S.op("act", lambda e, Pg=Pg, sg=sg, N=N: e.activation(out=sg[:, 0:N], in_=Pg[:, 0:N], func=AF.Sigmoid),
                     reads=[Pg], writes=[sg])
                S.op("dve", lambda e, Pa=Pa, sg=sg, N=N, off=off, cc=cc: e.tensor_tensor(
                    out=u[:, cc, off:off + N], in0=Pa[:, 0:N], in1=sg[:, 0:N], op=ALU.mult), reads=[Pa, sg], writes=[u])
                if grp == 0:
                    S.op("dve", lambda e, cc=cc: e.tensor_scalar(out=u[:, cc, 0:32], in0=u[:, cc, 0:32], scalar1=flag[:, 0:1],
                                                                 scalar2=None, op0=ALU.mult), reads=[u, flag], writes=[u])
        for cc in range(4):
            for tg in range(4):
                Pc = ring(pb, pi); pi += 1
                for k in range(31):
                    S.op("pe", lambda e, Pc=Pc, cc=cc, k=k, tg=tg: e.matmul(
                        Pc[:], lhsT=dgm[:, cc, k, :], rhs=u[:, cc, tg * 512 + 2 + k:tg * 512 + 2 + k + 512], start=(k == 0), stop=(k == 30)),
                        reads=[dgm, u], writes=[Pc])
                S.op("act", lambda e, Pc=Pc, cc=cc, tg=tg: e.activation(out=y[:, cc, tg * 512:(tg + 1) * 512], in_=Pc[:], func=AF.Identity,
                                                                       bias=cvec[:, 0, cc:cc + 1]), reads=[Pc, cvec], writes=[y])
        onesf = C["onesf512"]
        ysq = [S.sbuf("ysq", [128, 512], F32) for _ in range(2)]
        mean = S.sbuf("mean", [128, 512], F32)
        msq = S.sbuf("msq", [128, 512], F32)
        rstd = S.sbuf("rstdc", [128, 512], F32)
        t1 = [S.sbuf("t1", [128, 512], F32) for _ in range(2)]
        sT = S.sbuf("sT", [128, 4, 512], BF16)
        cvT = S.sbuf("cvT", [128, 4, TOK], BF16)
        for tg in range(4):
            ts_ = slice(tg * 512, (tg + 1) * 512)
            Pm = ring(pb, pi); Pq = ring(pb, pi + 1); pi += 2
            for cc in range(4):
                S.op("pe", lambda e, Pm=Pm, cc=cc, ts_=ts_: e.matmul(Pm[:], lhsT=onesf[:], rhs=y[:, cc, ts_], start=(cc == 0), stop=(cc == 3)),
                     reads=[onesf, y], writes=[Pm])
            for cc in range(4):
                yq = ring(ysq, cc)
                S.op("act", lambda e, yq=yq, cc=cc, ts_=ts_: e.activation(out=yq[:], in_=y[:, cc, ts_], func=AF.Square), reads=[y], writes=[yq])
                S.op("pe", lambda e, Pq=Pq, yq=yq, cc=cc: e.matmul(Pq[:], lhsT=onesf[:], rhs=yq[:], start=(cc == 0), stop=(cc == 3)),
                     reads=[onesf, yq], writes=[Pq])
            S.op("dve", lambda e, Pm=Pm: e.tensor_copy(out=mean[:], in_=Pm[:]), reads=[Pm], writes=[mean])
            S.op("dve", lambda e: e.tensor_tensor(out=msq[:], in0=mean[:], in1=mean[:], op=ALU.mult), reads=[mean], writes=[msq])
            S.op("dve", lambda e, Pq=Pq: e.tensor_tensor(out=msq[:], in0=Pq[:], in1=msq[:], op=ALU.subtract), reads=[Pq, msq], writes=[msq])
            S.op("act", lambda e: e.activation(out=rstd[:], in_=msq[:], func=AF.Sqrt, bias=C["eps"][:, 0:1]), reads=[msq, C["eps"]], writes=[rstd])
            S.op("dve", lambda e: e.reciprocal(out=rstd[:], in_=rstd[:]), reads=[rstd], writes=[rstd])
            for cc in range(4):
                tt1 = ring(t1, cc)
                S.op("dve", lambda e, tt1=tt1, cc=cc, ts_=ts_: e.tensor_tensor(out=tt1[:], in0=y[:, cc, ts_], in1=mean[:], op=ALU.subtract),
                     reads=[y, mean], writes=[tt1])
                S.op("dve", lambda e, tt1=tt1: e.tensor_tensor(out=tt1[:], in0=tt1[:], in1=rstd[:], op=ALU.mult), reads=[tt1, rstd], writes=[tt1])
                S.op("act", lambda e, tt1=tt1, cc=cc: e.activation(out=sT[:, cc, :], in_=tt1[:], func=AF.Silu,
                                                                 scale=cvec[:, 1, cc:cc + 1], bias=cvec[:, 2, cc:cc + 1]),
                     reads=[tt1, cvec], writes=[sT])
            for co in range(4):
                Pc = ring(pb, pi); pi += 1
                for ci in range(4):
                    S.op("pe", lambda e, Pc=Pc, ci=ci, co=co: e.matmul(Pc[:], lhsT=wpw[:, ci, co * 128:(co + 1) * 128], rhs=sT[:, ci, :],
                                                                     start=(ci == 0), stop=(ci == 3)), reads=[wpw, sT], writes=[Pc])
                evac(S, co, cvT[:, co, ts_], cvT, Pc, Pc[:])
        S.op("sp", lambda e: e.dma_start(out=cvT_dram.rearrange("(c p) t -> p c t", p=128), in_=cvT[:]), reads=[cvT], dma="st_cv")
        S.wait_all("sp")


def phase_outproj(S, C, x_dram, oT_attn, cvT_dram, w_out, g2_dram, x1_dram, h2T_dram, wr_dram=None, comb_dram=None, oload=None, omap=None):
    moe = wr_dram is not None
    with S.scope():
        pb = [S.psum("pbo", [128, 512], F32) for _ in range(4)]
        pst = [S.psum("pst", [128, 1024], BF16) for _ in range(2)]
        pf = [S.psum("pbf", [128, 512], F32) for _ in range(2)] if moe else None
        Cn = norm_ctx(S, C)
        wo = S.sbuf("wo", [128, 16, D], BF16)
        with S.scope():
            Cw = {"wstage": [S.sbuf("wstage", [128, 4096], F32) for _ in range(3)]}
            for i in range(8):
                load_cast(S, Cw, wo, wo[:, :, i * 256:(i + 1) * 256],
                          w_out[:, i * 256:(i + 1) * 256].rearrange("(c p) n -> p c n", p=128), [128, 16, 256], i,
                          eng=["dve", "act", "pool"][i % 3])
        gbc = S.sbuf("gbc", [128, D], F32)
        S.op("sp", lambda e: e.dma_start(out=gbc[:], in_=g2_dram.partition_broadcast(128)), writes=[gbc], dma="ld_c_g")
        if moe:
            wr = S.sbuf("wr", [128, 16, NE], F32)
            S.op("sp", lambda e: e.dma_start(out=wr[:], in_=wr_dram.rearrange("(c p) n -> p c n", p=128)), writes=[wr], dma="ld_c_wr")
            comb = S.sbuf("comb", [128, TOK // 128, NE], F32)
            hf = S.sbuf("hf", [128, D], F32)
            hTf = S.sbuf("hTf", [128, 16, 128], F32)
            lg = S.sbuf("lg", [128, NE], F32)
            top8 = S.sbuf("top8", [128, 8], F32)
            sc = S.sbuf("rsc", [128, 8], F32)
            m1t = S.sbuf("m1t", [128, NE], F32)
            m2t = S.sbuf("m2t", [128, NE], F32)
        xs = [S.sbuf("xs", [128, D], F32) for _ in range(2)]
        ots = [S.sbuf("ots", [128, 16, 512], BF16) for _ in range(2)]
        hst = [S.sbuf("hst", [128, 16, 512], BF16) for _ in range(2)]
        ov = oT_attn.rearrange("(c p) t -> p c t", p=128) if oT_attn is not None else None
        cv = cvT_dram.rearrange("(c p) t -> p c t", p=128)
        hT_v = h2T_dram.rearrange("(c p) t -> p c t", p=128)
        pi = 0
        def ldo(g):
            ot = ring(ots, g)
            gs = slice(g * 512, (g + 1) * 512)
            if oload is not None:
                S.op("pool", lambda e, ot=ot, g=g, gs=gs: oload(e, ot, g) + [e.dma_start(out=ot[:, 6:10, :], in_=cv[:, :, gs])],
                     writes=[ot], dma=f"ld_o{g % 2}", n=13)
                return
            mp = omap if omap is not None else [(0, 6, 0), (10, 6, 6)]
            S.op("sp", lambda e, ot=ot, gs=gs: [e.dma_start(out=ot[:, c0:c0 + n_, :], in_=ov[:, s0:s0 + n_, gs]) for (c0, n_, s0) in mp]
                 + [e.dma_start(out=ot[:, 6:10, :], in_=cv[:, :, gs])],
                 writes=[ot], dma=f"ld_o{g % 2}", n=len(mp) + 1)
        def ldx(tt):
            xt = ring(xs, tt)
            S.op("sp", lambda e, xt=xt, tt=tt: e.dma_start(out=xt[:], in_=x_dram[tt * 128:(tt + 1) * 128, :]),
                 writes=[xt], dma=f"ld_x{tt % 2}")
        ldo(0); ldx(0)
        for tt in range(TOK // 128):
            g = tt // 4
            ot = ring(ots, g)
            if tt % 4 == 0 and g + 1 < 4:
                ldo(g + 1)
            xt = ring(xs, tt)
            if tt + 1 < TOK // 128:
                ldx(tt + 1)
            tl = slice((tt % 4) * 128, (tt % 4 + 1) * 128)
            for cg in range(4):
                P = ring(pb, pi); pi += 1
                for c in range(16):
                    S.op("pe", lambda e, P=P, c=c, cg=cg, ot=ot, tl=tl: e.matmul(
                        P[:], lhsT=ot[:, c, tl], rhs=wo[:, c, cg * 512:(cg + 1) * 512], start=(c == 0), stop=(c == 15)),
                        reads=[ot, wo], writes=[P])
                S.op("dve", lambda e, P=P, xt=xt, cg=cg: e.tensor_tensor(out=xt[:, cg * 512:(cg + 1) * 512], in0=P[:],
                                                                        in1=xt[:, cg * 512:(cg + 1) * 512], op=ALU.add),
                     reads=[P, xt], writes=[xt])
            S.op("sp", lambda e, xt=xt, tt=tt: e.dma_start(out=x1_dram[tt * 128:(tt + 1) * 128, :], in_=xt[:]),
                 reads=[xt], dma=f"st_x{tt % 2}")
            hstage = ring(hst, g)
            emit_norm_tile(S, Cn, xt, gbc, hstage, (tt % 4) * 128, pst, tt, extra=(hf if moe else None))
            if tt % 4 == 3:
                S.op("sp", lambda e, hstage=hstage, g=g: e.dma_start(out=hT_v[:, :, g * 512:(g + 1) * 512], in_=hstage[:]),
                     reads=[hstage], dma=f"st_h{g % 2}")
            if moe:
                identf = C["ident_f"]
                for q4 in range(4):
                    Pf = ring(pf, q4)
                    for k in range(4):
                        c = q4 * 4 + k
                        S.op("pe", lambda e, Pf=Pf, c=c, k=k: e.transpose(out=Pf[:, k * 128:(k + 1) * 128], in_=hf[:, c * 128:(c + 1) * 128],
                                                                           identity=identf[:]), reads=[hf, identf], writes=[Pf])
                    evac(S, q4, hTf[:, q4 * 4:(q4 + 1) * 4, :], hTf, Pf, Pf[:].rearrange("p (c t) -> p c t", c=4))
                Pl = ring(pb, pi); pi += 1
                for c in range(16):
                    S.op("pe", lambda e, Pl=Pl, c=c: e.matmul(Pl[:, 0:NE], lhsT=hTf[:, c, :], rhs=wr[:, c, :], start=(c == 0), stop=(c == 15)),
                         reads=[hTf, wr], writes=[Pl])
                S.op("dve", lambda e, Pl=Pl: e.tensor_copy(out=lg[:], in_=Pl[:, 0:NE]), reads=[Pl], writes=[lg])
                S.op("dve", lambda e: e.max(out=top8[:], in_=lg[:]), reads=[lg], writes=[top8])
                S.op("dve", lambda e: e.tensor_tensor(out=sc[:, 0:1], in0=top8[:, 1:2], in1=top8[:, 0:1], op=ALU.subtract), reads=[top8], writes=[sc])
                S.op("act", lambda e: e.activation(out=sc[:, 1:2], in_=sc[:, 0:1], func=AF.Exp), reads=[sc], writes=[sc])
                S.op("dve", lambda e: e.tensor_scalar(out=sc[:, 2:3], in0=sc[:, 1:2], scalar1=1.0, scalar2=None, op0=ALU.add), reads=[sc], writes=[sc])
                S.op("dve", lambda e: e.reciprocal(out=sc[:, 2:3], in_=sc[:, 2:3]), reads=[sc], writes=[sc])
                S.op("dve", lambda e: e.tensor_tensor(out=sc[:, 3:4], in0=sc[:, 1:2], in1=sc[:, 2:3], op=ALU.mult), reads=[sc], writes=[sc])
                S.op("dve", lambda e: e.tensor_scalar(out=m1t[:], in0=lg[:], scalar1=top8[:, 0:1], scalar2=sc[:, 2:3],
                                                      op0=ALU.is_equal, op1=ALU.mult), reads=[lg, top8, sc], writes=[m1t])
                S.op("dve", lambda e: e.tensor_scalar(out=m2t[:], in0=lg[:], scalar1=top8[:, 1:2], scalar2=sc[:, 3:4],
                                                      op0=ALU.is_equal, op1=ALU.mult), reads=[lg, top8, sc], writes=[m2t])
                S.op("dve", lambda e, tt=tt: e.tensor_tensor(out=comb[:, tt, :], in0=m1t[:], in1=m2t[:], op=ALU.add), reads=[m1t, m2t], writes=[comb])
        if moe:
            S.op("sp", lambda e: e.dma_start(out=comb_dram.rearrange("(t p) n -> p t n", p=128), in_=comb[:]), reads=[comb], dma="st_cb")
        S.wait_all("sp")


FE = 2816
NFC = FE // 128
NFB = FE // 256


def phase_ffn(S, C, h2T_dram, x1_dram, x2_dram, experts, comb_dram, aT_scr):
    with S.scope():
        pg = [S.psum("pg", [128, 512], F32) for _ in range(2)]
        pu = [S.psum("pu", [128, 512], F32) for _ in range(2)]
        py = [S.psum("py", [128, 512], F32) for _ in range(2)]
        C = dict(C)
        C["wstage"] = [S.sbuf("wstage", [128, 4096], F32) for _ in range(2)]
        h2T = S.sbuf("h2T", [128, 16, TOK], BF16)
        hv = h2T_dram.rearrange("(c p) t -> p c t", p=128)
        for g in range(4):
            S.op("sp", lambda e, g=g: e.dma_start(out=h2T[:, :, g * 512:(g + 1) * 512], in_=hv[:, :, g * 512:(g + 1) * 512]),
                 writes=[h2T], dma="ld_c_h2")
        use_comb = any(ex[3] is not None for ex in experts)
        if use_comb:
            comb = S.sbuf("combf", [128, TOK // 128, NE], F32)
            S.op("sp", lambda e: e.dma_start(out=comb[:], in_=comb_dram.rearrange("(t p) n -> p t n", p=128)), writes=[comb], dma="ld_c_cb")
        wgs = [S.sbuf("wgs", [128, 16, 128], BF16) for _ in range(2)]
        wus = [S.sbuf("wus", [128, 16, 128], BF16) for _ in range(2)]
        wds = [S.sbuf("wds", [128, NFC, 512], BF16) for _ in range(2)]
        sgs = [S.sbuf("sgf", [128, 512], F32) for _ in range(2)]
        ast = [S.sbuf("ast", [128, TOK], BF16) for _ in range(2)]
        ats = [S.sbuf("ats", [128, NFC, 128], BF16) for _ in range(3)]
        accs = [S.sbuf("acc", [128, 512], F32) for _ in range(3)]
        li = 0
        pi = 0
        NCG = D // 512
        jobs = [(cg, tt) for cg in range(NCG) for tt in range(TOK // 128)]
        for ei_, (wg_ap, wu_ap, wd_ap, ccol) in enumerate(experts):
            src_x = x1_dram if ei_ == 0 else x2_dram

            def ldw(fb, li, wg_ap=wg_ap, wu_ap=wu_ap):
                if fb >= NFC:
                    if ei_ + 1 >= len(experts):
                        return
                    wg_ap, wu_ap = experts[ei_ + 1][0], experts[ei_ + 1][1]
                    fb = 0
                wgb = ring(wgs, fb); wub = ring(wus, fb)
                fs = slice(fb * 128, (fb + 1) * 128)
                load_cast(S, C, wgb, wgb[:], wg_ap[:, fs].rearrange("(c p) n -> p c n", p=128), [128, 16, 128], li)
                load_cast(S, C, wub, wub[:], wu_ap[:, fs].rearrange("(c p) n -> p c n", p=128), [128, 16, 128], li + 1)

            def ldwd(cg, li, wd_ap=wd_ap):
                wdb = ring(wds, cg)
                cs = slice(cg * 512, (cg + 1) * 512)
                for k, (f0, nf) in enumerate(((0, 8), (8, 8), (16, 6))):
                    load_cast(S, C, wdb, wdb[:, f0:f0 + nf, :],
                              wd_ap[f0 * 128:(f0 + nf) * 128, cs].rearrange("(f p) n -> p f n", p=128), [128, nf, 512], li + k,
                              eng="act")

            if ei_ > 0:
                S.wait_all("sp", keys=[k for k in S.cnt if k.startswith("ld_a")])
            if ei_ == 0:
                ldw(0, li); li += 2
            for fb in range(NFC):
                wgb = ring(wgs, fb); wub = ring(wus, fb)
                if fb + 1 < NFC:
                    ldw(fb + 1, li); li += 2
                if fb == NFC - 2:
                    ldwd(0, li); li += 3
                a_st = ring(ast, fb)
                for tg in range(4):
                    G = ring(pg, pi); U = ring(pu, pi); pi += 1
                    ts_ = slice(tg * 512, (tg + 1) * 512)
                    for c in range(16):
                        S.op("pe", lambda e, G=G, c=c, wgb=wgb, ts_=ts_: e.matmul(
                            G[:], lhsT=wgb[:, c, :], rhs=h2T[:, c, ts_], start=(c == 0), stop=(c == 15)),
                            reads=[wgb, h2T], writes=[G])
                    for c in range(16):
                        S.op("pe", lambda e, U=U, c=c, wub=wub, ts_=ts_: e.matmul(
                            U[:], lhsT=wub[:, c, :], rhs=h2T[:, c, ts_], start=(c == 0), stop=(c == 15)),
                            reads=[wub, h2T], writes=[U])
                    sg = ring(sgs, pi)
                    S.op("act", lambda e, G=G, sg=sg: e.activation(out=sg[:], in_=G[:], func=AF.Silu), reads=[G], writes=[sg])
                    S.op("dve", lambda e, U=U, sg=sg, a_st=a_st, ts_=ts_: e.tensor_tensor(
                        out=a_st[:, ts_], in0=U[:], in1=sg[:], op=ALU.mult), reads=[U, sg], writes=[a_st])
                S.op("sp", lambda e, a_st=a_st, fb=fb: e.dma_start(
                    out=aT_scr[:, :, fb, :].rearrange("t p k -> p t k"),
                    in_=a_st[:].rearrange("p (t k) -> p t k", k=128)), reads=[a_st], dma=f"st_a{fb % 2}")
            S.wait_all("sp", keys=[k for k in S.cnt if k.startswith("st_a") or k.startswith("st_y")])

            def ldat(i):
                cg, tt = jobs[i]
                at = ring(ats, i)
                S.op("sp", lambda e, at=at, tt=tt: e.dma_start(out=at[:], in_=aT_scr[tt]), writes=[at], dma=f"ld_a{i % 3}")

            def ldacc(i, src_x=src_x):
                cg, tt = jobs[i]
                cs = slice(cg * 512, (cg + 1) * 512)
                acc = ring(accs, i)
                S.op("sp", lambda e, acc=acc, tt=tt, cs=cs, src_x=src_x: e.dma_start(out=acc[:], in_=src_x[tt * 128:(tt + 1) * 128, cs]),
                     writes=[acc], dma=f"ld_y{i % 3}")
            ldat(0); ldat(1); ldacc(0); ldacc(1)
            for i, (cg, tt) in enumerate(jobs):
                wdb = ring(wds, cg)
                cs = slice(cg * 512, (cg + 1) * 512)
                if tt == 0 and cg + 1 < NCG:
                    ldwd(cg + 1, li); li += 3
                if i == len(jobs) - 6:
                    ldw(NFC, li); li += 2
                if i + 2 < len(jobs):
                    ldat(i + 2)
                if i + 2 < len(jobs):
                    ldacc(i + 2)
                at = ring(ats, i); acc = ring(accs, i)
                Y = ring(py, i)
                for f in range(NFC):
                    S.op("pe", lambda e, Y=Y, f=f, at=at, wdb=wdb: e.matmul(Y[:], lhsT=at[:, f, :], rhs=wdb[:, f, :],
                                                                           start=(f == 0), stop=(f == NFC - 1)),
                         reads=[at, wdb], writes=[Y])
                if ccol is None:
                    S.op("dve", lambda e, Y=Y, acc=acc: e.tensor_tensor(out=acc[:], in0=Y[:], in1=acc[:], op=ALU.add),
                         reads=[Y, acc], writes=[acc])
                else:
                    S.op("dve", lambda e, Y=Y, acc=acc, tt=tt, ccol=ccol: e.scalar_tensor_tensor(
                        out=acc[:], in0=Y[:], scalar=comb[:, tt, ccol:ccol + 1], in1=acc[:], op0=ALU.mult, op1=ALU.add),
                        reads=[Y, acc, comb], writes=[acc])
                S.op("sp", lambda e, acc=acc, tt=tt, cs=cs: e.dma_start(out=x2_dram[tt * 128:(tt + 1) * 128, cs], in_=acc[:]),
                     reads=[acc], dma=f"st_y{i % 3}")
        S.barrier()


def phase_final_norm(S, C, x_dram, g_dram, out_dram):
    with S.scope():
        Cn = norm_ctx(S, C)
        gbc = S.sbuf("gbc", [128, D], F32)
        S.op("sp", lambda e: e.dma_start(out=gbc[:], in_=g_dram.partition_broadcast(128)), writes=[gbc], dma="ld_c_g")
        xs = [S.sbuf("xs", [128, D], F32) for _ in range(2)]
        os_ = [S.sbuf("os", [128, D], F32) for _ in range(2)]
        for tt in range(TOK // 128):
            xt = ring(xs, tt); ot = ring(os_, tt)
            junk = Cn["junk"][0]; ss = ring(Cn["ss"], tt); rstd = ring(Cn["rstd"], tt)
            S.op("sp", lambda e, xt=xt, tt=tt: e.dma_start(out=xt[:], in_=x_dram[tt * 128:(tt + 1) * 128, :]), writes=[xt], dma=f"ld_x{tt % 2}")
            S.op("act", lambda e, xt=xt, ss=ss: e.activation(out=junk[:], in_=xt[:], func=AF.Square, accum_out=ss[:]), reads=[xt], writes=[junk, ss])
            S.op("act", lambda e, ss=ss, rstd=rstd: e.activation(out=rstd[:], in_=ss[:], func=AF.Sqrt, scale=1.0 / D, bias=Cn["eps"][:, 0:1]),
                 reads=[ss, Cn["eps"]], writes=[rstd])
            S.op("dve", lambda e, rstd=rstd: e.reciprocal(out=rstd[:], in_=rstd[:]), reads=[rstd], writes=[rstd])
            S.op("dve", lambda e, xt=xt, ot=ot, rstd=rstd: e.scalar_tensor_tensor(out=ot[:], in0=xt[:], scalar=rstd[:, 0:1], in1=gbc[:],
                                                                              op0=ALU.mult, op1=ALU.mult), reads=[xt, rstd, gbc], writes=[ot])
            S.op("sp", lambda e, ot=ot, tt=tt: e.dma_start(out=out_dram[tt * 128:(tt + 1) * 128, :], in_=ot[:]), reads=[ot], dma=f"st_f{tt % 2}")
        S.wait_all("sp")


from concourse.bass_utils import run_bass_kernel_spmd


def _ext(nc, name, shape, dt_, out=False):
    return nc.dram_tensor(name, list(shape), dt_, kind="ExternalOutput" if out else "ExternalInput").ap()


DEBUG_EXT = False


def _int(nc, name, shape, dt_):
    return nc.dram_tensor(name, list(shape), dt_, kind="ExternalOutput" if DEBUG_EXT else "Internal").ap()


def build_N():
    nc = bass.Bass("TRN2", target_bir_lowering=False)
    x = _ext(nc, "x", [TOK, D], F32)
    g = _ext(nc, "g", [D], F32)
    hT = _ext(nc, "hT", [D, TOK], BF16, out=True)
    S = Sched(nc)
    C = declare_consts(nc, S, ["ident_bf"])
    phase_norm(S, C, x, g, hT)
    S.barrier()
    S.emit()
    return nc


def build_H(layer):
    nc = bass.Bass("TRN2", target_bir_lowering=False)
    hT = _ext(nc, "hT_full", [D, SEQ], BF16)
    w_sb = _ext(nc, "w_sb", [D, 1152], F32)
    w_da = _ext(nc, "w_da", [D, 1152], F32)
    lam = _ext(nc, "lam", [4, 64], F32)
    dg = _ext(nc, "diffg", [128, 3], F32)
    oT = _ext(nc, "oT", [768, SEQ], BF16, out=True)
    S = Sched(nc)
    C = declare_consts(nc, S, ["ident_bf", "ones_bf", "negones", "negtri", "onesf128", "sbmask", "dabias", "damask"])
    hv = hT.rearrange("(c p) t -> p c t", p=128)
    phase_heads(S, C, lambda tg: [(0, 16, hv[:, :, tg * 256:(tg + 1) * 256])], w_sb, w_da, lam, dg, oT, layer)
    S.barrier()
    S.emit()
    return nc


def build_T(layer, last):
    moe = (layer % 2 == 1)
    nc = bass.Bass("TRN2", target_bir_lowering=False)
    x = _ext(nc, "x", [TOK, D], F32)
    hT_own = _ext(nc, "hT_own", [D, TOK], BF16)
    hT_halo = _ext(nc, "hT_halo", [D, 32], BF16)
    flag = _ext(nc, "flag", [128, 1], F32)
    oT_attn = _ext(nc, "oT_attn", [1536, TOK], BF16)
    w_glu = _ext(nc, "w_glu", [D, 1024], F32)
    cw = _ext(nc, "cw", [128, 4, 31], F32)
    cvec = _ext(nc, "cvec", [128, 3, 4], F32)
    w_pw = _ext(nc, "w_pw", [512, 512], F32)
    w_out = _ext(nc, "w_out", [D, D], F32)
    g2 = _ext(nc, "g2", [D], F32)
    gn = _ext(nc, "g_next", [D], F32)
    if moe:
        wr = _ext(nc, "w_router", [D, NE], F32)
        eg = _ext(nc, "e_gate", [NE, D, FE], F32)
        eu = _ext(nc, "e_up", [NE, D, FE], F32)
        ed = _ext(nc, "e_down", [NE, FE, D], F32)
        experts = [(eg[e], eu[e], ed[e], e) for e in range(NE)]
    else:
        wgt = _ext(nc, "w_gate", [D, D_FF], F32)
        wup = _ext(nc, "w_up", [D, D_FF], F32)
        wdn = _ext(nc, "w_down", [D_FF, D], F32)
        experts = [(wgt[:, h * FE:(h + 1) * FE], wup[:, h * FE:(h + 1) * FE], wdn[h * FE:(h + 1) * FE, :], None) for h in range(2)]
    cvT = _int(nc, "cvT", [512, TOK], BF16)
    x1 = _int(nc, "x1", [TOK, D], F32)
    h2T = _int(nc, "h2T", [D, TOK], BF16)
    comb = _int(nc, "comb", [TOK, NE], F32)
    aT_scr = _int(nc, "aT_scr", [16, 128, NFC, 128], BF16)
    if last:
        x2 = _int(nc, "x2", [TOK, D], F32)
        out = _ext(nc, "out", [TOK, D], F32, out=True)
    else:
        x2 = _ext(nc, "x2", [TOK, D], F32, out=True)
        hT_next = _ext(nc, "hT_next", [D, TOK], BF16, out=True)
    S = Sched(nc)
    C = declare_consts(nc, S, ["ident_bf", "ident_f", "onesf512"])
    C["eps"] = S.sbuf("epsc", [128, 1], F32)
    S.op("pool", lambda e: e.memset(C["eps"][:], EPS), writes=[C["eps"]])
    S.barrier()
    phase_conv(S, C, hT_own, hT_halo, flag, w_glu, cw, cvec, w_pw, cvT)
    S.barrier()
    phase_outproj(S, C, x, oT_attn, cvT, w_out, g2, x1, h2T, wr_dram=(wr if moe else None), comb_dram=(comb if moe else None))
    S.barrier()
    if "ffn" not in DEBUG_SKIP:
        phase_ffn(S, C, h2T, x1, x2, experts, comb, aT_scr)
    S.barrier()
    if last:
        phase_final_norm(S, C, x2, gn, out)
    else:
        with S.scope():
            phase_norm(S, C, x2, gn, hT_next)
    S.barrier()
    S.emit()
    return nc


RG_PAIR = [[0, 1], [2, 3], [4, 5], [6, 7]]
DEPTH = 2
H_CONSTS = ["ident_bf", "ident_f", "ones_bf", "negones", "negtri", "onesf128", "onesf512", "sbmask", "dabias", "damask"]


def build_fused():
    nc = bass.Bass("TRN2", target_bir_lowering=False)
    x_in = _ext(nc, "x", [TOK, D], F32)
    out = _ext(nc, "out", [TOK, D], F32, out=True)
    flag = _ext(nc, "flag", [128, 1], F32)
    g_attn = [_ext(nc, f"g_attn{l}", [D], F32) for l in range(DEPTH)]
    g_ffn = [_ext(nc, f"g_ffn{l}", [D], F32) for l in range(DEPTH)]
    g_fin = _ext(nc, "g_final", [D], F32)
    w_sb = [_ext(nc, f"w_sb{l}", [D, 1152], F32) for l in range(DEPTH)]
    w_da = [_ext(nc, f"w_da{l}", [D, 1152], F32) for l in range(DEPTH)]
    lam = [_ext(nc, f"lam{l}", [4, 64], F32) for l in range(DEPTH)]
    dg = [_ext(nc, f"diffg{l}", [128, 3], F32) for l in range(DEPTH)]
    w_glu = [_ext(nc, f"w_glu{l}", [D, 1024], F32) for l in range(DEPTH)]
    cw = [_ext(nc, f"cw{l}", [128, 4, 31], F32) for l in range(DEPTH)]
    cvec = [_ext(nc, f"cvec{l}", [128, 3, 4], F32) for l in range(DEPTH)]
    w_pw = [_ext(nc, f"w_pw{l}", [512, 512], F32) for l in range(DEPTH)]
    w_out = [_ext(nc, f"w_out{l}", [D, D], F32) for l in range(DEPTH)]
    wgt = _ext(nc, "w_gate", [D, D_FF], F32)
    wup = _ext(nc, "w_up", [D, D_FF], F32)
    wdn = _ext(nc, "w_down", [D_FF, D], F32)
    wr = _ext(nc, "w_router", [D, NE], F32)
    eg = _ext(nc, "e_gate", [NE, D, FE], F32)
    eu = _ext(nc, "e_up", [NE, D, FE], F32)
    ed = _ext(nc, "e_down", [NE, FE, D], F32)
    hT_own_t = nc.dram_tensor("hT_own", [D, TOK], BF16)
    hT_g_t = nc.dram_tensor("hT_g", [2 * D, TOK], BF16)
    oT_h_t = nc.dram_tensor("oT_h", [768, SEQ], BF16)
    oT_g_t = nc.dram_tensor("oT_g", [1536, SEQ], BF16)
    hT_own, hT_g, oT_h, oT_g = hT_own_t.ap(), hT_g_t.ap(), oT_h_t.ap(), oT_g_t.ap()
    cvT = _int(nc, "cvT", [512, TOK], BF16)
    x1 = _int(nc, "x1", [TOK, D], F32)
    h2T = _int(nc, "h2T", [D, TOK], BF16)
    comb = _int(nc, "comb", [TOK, NE], F32)
    aT_scr = _int(nc, "aT_scr", [16, 128, NFC, 128], BF16)
    xA = _int(nc, "xA", [TOK, D], F32)
    xB = _int(nc, "xB", [TOK, D], F32)

    S = Sched(nc)
    C = declare_consts(nc, S, H_CONSTS)
    C["eps"] = S.sbuf("epsc", [128, 1], F32)
    S.op("pool", lambda e: e.memset(C["eps"][:], EPS), writes=[C["eps"]])
    S.barrier()
    with S.scope():
        phase_norm(S, C, x_in, g_attn[0], hT_own)
    S.barrier()
    ncc = [0]

    def allgather(src_t, dst_t, npieces, rows):
        S.barrier()
        for k in range(npieces):
            S.op("pool", lambda e, k=k: e.collective_compute(
                "AllGather", ALU.bypass, replica_groups=RG_PAIR,
                ins=[src_t.ap()[k * rows:(k + 1) * rows, :].opt()], outs=[dst_t.ap()[2 * k * rows:2 * (k + 1) * rows, :].opt()]),
                dma="cc", inc=1)
        S.barrier()

    def hsrc(tg):
        rank, loc = tg // 8, tg % 8
        return [(4 * k, 4, hT_g[k * 1024 + rank * 512:k * 1024 + (rank + 1) * 512, loc * 256:(loc + 1) * 256].rearrange("(c p) t -> p c t", p=128))
                for k in range(4)]

    halo = [(4 * k, 4, hT_g[k * 1024:k * 1024 + 512, TOK - 32:TOK]) for k in range(4)]

    oT_mine = _int(nc, "oT_mine", [1536, TOK], BF16)
    rowchunk = lambda rank, fc: (fc // 2) * 4 + rank * 2 + (fc % 2)
    OMAP = [(h, 1, rowchunk(h // 3, h % 3)) for h in range(6)] + [(10 + h, 1, rowchunk(h // 3, 3 + h % 3)) for h in range(6)]

    def copy_mine():
        def f(e):
            pid = e.partition_id()
            r = pid % 2
            return e.dma_start(out=oT_mine, in_=oT_g[:, bass.ds(r * TOK, TOK)])
        S.op("pool", f, dma="cp_o")
        S.barrier()

    x_cur = x_in
    for l in range(DEPTH):
        last = (l == DEPTH - 1)
        moe = (l % 2 == 1)
        allgather(hT_own_t, hT_g_t, 4, 512)
        with S.scope():
            phase_heads(S, C, hsrc, w_sb[l], w_da[l], lam[l], dg[l], oT_h, l)
        allgather(oT_h_t, oT_g_t, 3, 256)
        copy_mine()
        phase_conv(S, C, hT_own, halo, flag, w_glu[l], cw[l], cvec[l], w_pw[l], cvT)
        S.barrier()
        phase_outproj(S, C, x_cur, oT_mine, cvT, w_out[l], g_ffn[l], x1, h2T,
                      wr_dram=(wr if moe else None), comb_dram=(comb if moe else None), omap=OMAP)
        S.barrier()
        x2 = xB if moe else xA
        if moe:
            experts = [(eg[e], eu[e], ed[e], e) for e in range(NE)]
        else:
            experts = [(wgt[:, h * FE:(h + 1) * FE], wup[:, h * FE:(h + 1) * FE], wdn[h * FE:(h + 1) * FE, :], None) for h in range(2)]
        phase_ffn(S, C, h2T, x1, x2, experts, comb, aT_scr)
        S.barrier()
        if last:
            phase_final_norm(S, C, x2, g_fin, out)
        else:
            with S.scope():
                phase_norm(S, C, x2, g_attn[l + 1], hT_own)
        S.barrier()
        x_cur = x2
    S.emit()
    return nc


def fused_inputs(c, x, attn_norm, w_in, w_out, lam, diff_norm, conv_w, conv_b, conv_ln_g, conv_ln_b,
                 w_conv_out, ffn_norm, w_gate, w_up, w_down, w_router, e_gate, e_up, e_down, final_norm, shared):
    A = lambda a: np.ascontiguousarray(np.asarray(a, dtype=np.float32))
    b, r = c // 2, c % 2
    hcst = host_consts(r)
    m = {"x": A(x[b, r * TOK:(r + 1) * TOK]), "flag": np.full((128, 1), float(r), np.float32)}
    for k in H_CONSTS:
        m[k] = hcst[k]
    hs = [3 * r + i for i in range(3)]
    cols = lambda base: np.concatenate([np.arange(base + h * 128, base + (h + 1) * 128) for h in hs])
    for l in range(DEPTH):
        wl = np.asarray(w_in[l])
        m[f"w_sb{l}"] = A(np.concatenate([wl[:, cols(0)], wl[:, cols(768)], wl[:, cols(1536)]], axis=1))
        m[f"w_da{l}"] = A(np.concatenate([wl[:, cols(3328)], wl[:, cols(4096)], wl[:, cols(4864)]], axis=1))
        m[f"diffg{l}"] = A(np.asarray(diff_norm[l]).reshape(6, 128)[3 * r:3 * r + 3].T)
    m.update(shared)
    return m


def shared_inputs(attn_norm, w_in, w_out, lam, conv_w, conv_b, conv_ln_g, conv_ln_b,
                  w_conv_out, ffn_norm, w_gate, w_up, w_down, w_router, e_gate, e_up, e_down, final_norm):
    A = lambda a: np.ascontiguousarray(np.asarray(a, dtype=np.float32))
    m = {"g_final": A(final_norm), "w_gate": A(w_gate[0]), "w_up": A(w_up[0]), "w_down": A(w_down[0]),
         "w_router": A(w_router[0]), "e_gate": A(e_gate[0]), "e_up": A(e_up[0]), "e_down": A(e_down[0])}
    for l in range(DEPTH):
        wl = np.asarray(w_in[l])
        m[f"g_attn{l}"] = A(attn_norm[l]); m[f"g_ffn{l}"] = A(ffn_norm[l]); m[f"lam{l}"] = A(lam[l])
        m[f"w_glu{l}"] = A(wl[:, 2304:3328])
        m[f"cw{l}"] = A(np.asarray(conv_w[l]).T.reshape(4, 128, 31).transpose(1, 0, 2))
        m[f"cvec{l}"] = A(np.stack([np.asarray(conv_b[l]).reshape(4, 128).T, np.asarray(conv_ln_g[l]).reshape(4, 128).T,
                                    np.asarray(conv_ln_b[l]).reshape(4, 128).T], axis=1))
        m[f"w_pw{l}"] = A(w_conv_out[l]); m[f"w_out{l}"] = A(w_out[l])
    return m


_NC_CACHE = {}


def _get(name, fn):
    if name not in _NC_CACHE:
        _NC_CACHE[name] = fn()
    return _NC_CACHE[name]


def kernel(x, attn_norm, w_in, w_out, lam, diff_norm, conv_w, conv_b, conv_ln_g, conv_ln_b,
           w_conv_out, ffn_norm, w_gate, w_up, w_down, w_router, e_gate, e_up, e_down, final_norm):
    x = np.asarray(x, dtype=np.float32)
    shared = shared_inputs(attn_norm, w_in, w_out, lam, conv_w, conv_b, conv_ln_g, conv_ln_b,
                           w_conv_out, ffn_norm, w_gate, w_up, w_down, w_router, e_gate, e_up, e_down, final_norm)
    ins = [fused_inputs(c, x, attn_norm, w_in, w_out, lam, diff_norm, conv_w, conv_b, conv_ln_g, conv_ln_b,
                        w_conv_out, ffn_norm, w_gate, w_up, w_down, w_router, e_gate, e_up, e_down, final_norm, shared)
           for c in range(8)]
    nc = _get("fused", build_fused)
    res = run_bass_kernel_spmd(nc, ins, core_ids=list(range(8))).results
    out_full = np.zeros((NB, SEQ, D), np.float32)
    for c in range(8):
        out_full[c // 2, (c % 2) * TOK:(c % 2 + 1) * TOK] = res[c]["out"]
    return out_full
```
